# Optimizing a Trainium2 kernel written in Bass

```python
import math
import jax
import jax.numpy as jnp
from jax import lax
import numpy as np

D_MODEL = 1024
BATCH = 4
SEQ = 4096
DEPTH = 2

PLE_DIM = 256
N_EVEN = (DEPTH + 1) // 2
N_ODD = DEPTH // 2

DA_HEADS = 4
DA_HEAD_DIM = 64
DA_V_DIM = 2 * DA_HEAD_DIM
DA_QK_WIDTH = DA_HEADS * 2 * DA_HEAD_DIM
DA_WIDTH = DA_HEADS * DA_V_DIM
ROPE_THETA = 500000.0
ROT_DIM = DA_HEAD_DIM // 4
Q_BLOCK = 128

SC_WIDTH = 512
SC_WIDTH_CONV = 3
EVEN_IN_WIDTH = 3 * DA_QK_WIDTH // 2 * 2 // 2 * 0 + DA_QK_WIDTH * 2 + DA_WIDTH + 3 * SC_WIDTH

GM_WIDTH = D_MODEL
GM_GROUPS = 8
GM_GROUP_CH = GM_WIDTH // GM_GROUPS
GM_CHUNK = 128

D_FF = 2816
N_EXPERTS = 8
TOP_K = 2
MOE_BLOCK = 256

RMS_EPS = 1e-6
LN_EPS = 1e-5

kernel_name = "hybrid_diffattn_shortconv_gmlp_moe_ple"


def rms_norm(x, g):
    xf = x.astype(jnp.float32)
    y = xf * lax.rsqrt(jnp.mean(xf * xf, axis=-1, keepdims=True) + RMS_EPS)
    return (y * g.astype(jnp.float32)).astype(x.dtype)


def layer_norm(x, g, b):
    xf = x.astype(jnp.float32)
    mu = jnp.mean(xf, axis=-1, keepdims=True)
    var = jnp.mean(jnp.square(xf - mu), axis=-1, keepdims=True)
    y = (xf - mu) * lax.rsqrt(var + LN_EPS)
    return (y * g.astype(jnp.float32) + b.astype(jnp.float32)).astype(x.dtype)


def rope_partial(t, positions):
    half = ROT_DIM // 2
    inv_freq = ROPE_THETA ** (-jnp.arange(0, ROT_DIM, 2, dtype=jnp.float32) / ROT_DIM)
    ang = positions.astype(jnp.float32)[:, :, None] * inv_freq
    cos = jnp.cos(ang)[:, :, None, None, :].astype(t.dtype)
    sin = jnp.sin(ang)[:, :, None, None, :].astype(t.dtype)
    t1 = t[..., :half]
    t2 = t[..., half:ROT_DIM]
    return jnp.concatenate([t1 * cos - t2 * sin, t2 * cos + t1 * sin, t[..., ROT_DIM:]], axis=-1)


def diff_attention(q, k, v, lam, lambda_init, subln_g):
    bsz, seq = q.shape[0], q.shape[1]
    n_blocks = seq // Q_BLOCK
    q = q * (DA_HEAD_DIM ** -0.5)
    kpos = jnp.arange(seq)

    def one_block(i):
        qb = lax.dynamic_slice_in_dim(q, i * Q_BLOCK, Q_BLOCK, axis=1)
        s = jnp.einsum('bqhcd,bkhcd->bhcqk', qb, k, preferred_element_type=jnp.float32)
        qpos = i * Q_BLOCK + jnp.arange(Q_BLOCK)
        mask = kpos[None, :] <= qpos[:, None]
        pr = jax.nn.softmax(jnp.where(mask, s, -jnp.inf), axis=-1)
        w = pr[:, :, 0] - lam * pr[:, :, 1]
        return jnp.einsum('bhqk,bkhe->bqhe', w.astype(v.dtype), v)

    out = lax.map(one_block, jnp.arange(n_blocks))
    out = jnp.moveaxis(out, 0, 1).reshape(bsz, seq, DA_HEADS, DA_V_DIM)
    out = rms_norm(out, subln_g) * (1.0 - lambda_init)
    return out.reshape(bsz, seq, DA_WIDTH)


def causal_short_conv(z, w):
    ch = z.shape[-1]
    rhs = jnp.transpose(w).astype(z.dtype)[:, None, :]
    return lax.conv_general_dilated(z, rhs, window_strides=(1,), padding=[(SC_WIDTH_CONV - 1, 0)],
                                    dimension_numbers=('NWC', 'WIO', 'NWC'), feature_group_count=ch)


def spatial_gating(u, v, w_s, b_s):
    bsz, seq, _ = v.shape
    vr = v.reshape(bsz, seq // GM_CHUNK, GM_CHUNK, GM_GROUPS, GM_GROUP_CH)
    tril = jnp.tril(jnp.ones((GM_CHUNK, GM_CHUNK), dtype=w_s.dtype))
    vs = jnp.einsum('gts,bnsgc->bntgc', w_s * tril, vr)
    vs = vs + jnp.transpose(b_s)[None, None, :, :, None]
    return u * vs.reshape(bsz, seq, GM_WIDTH)


def swiglu(h, w1, w3, w2):
    return (jax.nn.silu(h @ w1) * (h @ w3)) @ w2


def moe_swiglu(h, router_w, w1, w3, w2):
    bsz, seq, dm = h.shape
    n_tok = bsz * seq
    n_assign = n_tok * TOP_K
    n_blocks = -(-(n_assign + N_EXPERTS * (MOE_BLOCK - 1)) // MOE_BLOCK)
    xt = h.reshape(n_tok, dm)
    logits = jnp.einsum('nd,de->ne', xt, router_w, preferred_element_type=jnp.float32)
    top_val, top_idx = lax.top_k(logits, TOP_K)
    gates = jax.nn.softmax(top_val, axis=-1)
    expert_flat = top_idx.reshape(-1)
    token_flat = jnp.arange(n_assign) // TOP_K
    gate_flat = gates.reshape(-1)
    order = jnp.argsort(expert_flat)
    sorted_expert = expert_flat[order]
    sorted_token = token_flat[order]
    counts = jnp.bincount(expert_flat, length=N_EXPERTS)
    starts = jnp.cumsum(counts) - counts
    padded = (counts + MOE_BLOCK - 1) // MOE_BLOCK * MOE_BLOCK
    pad_ends = jnp.cumsum(padded)
    pad_starts = pad_ends - padded
    dest = pad_starts[sorted_expert] + (jnp.arange(n_assign) - starts[sorted_expert])
    rows = jnp.zeros((n_blocks * MOE_BLOCK, dm), h.dtype).at[dest].set(xt[sorted_token])
    block_start = jnp.arange(n_blocks) * MOE_BLOCK
    block_expert = jnp.minimum(jnp.searchsorted(pad_ends, block_start, side='right'), N_EXPERTS - 1)

    def expert_block(args):
        xb, e = args
        return (jax.nn.silu(xb @ w1[e]) * (xb @ w3[e])) @ w2[e]

    y_rows = lax.map(expert_block, (rows.reshape(n_blocks, MOE_BLOCK, dm), block_expert))
    y_sorted = y_rows.reshape(n_blocks * MOE_BLOCK, dm)[dest]
    contrib = y_sorted * gate_flat[order][:, None].astype(h.dtype)
    out = jax.ops.segment_sum(contrib, sorted_token, num_segments=n_tok)
    return out.reshape(bsz, seq, dm)


def setup_inputs(seed: int = 0) -> dict:
    key = jax.random.key(seed)
    ks = list(jax.random.split(key, 40))
    cnt = [0]

    def nk():
        cnt[0] += 1
        return ks[cnt[0] - 1]

    def nrm(shape, scale):
        return scale * jax.random.normal(nk(), shape, jnp.float32)

    def gain(shape):
        return 1.0 + nrm(shape, 0.05)

    x = nrm((BATCH, SEQ, D_MODEL), 1.0)
    p = nrm((DEPTH, BATCH, SEQ, PLE_DIM), 1.0)
    positions = (jax.random.randint(nk(), (BATCH, 1), 0, 1024, jnp.int32)
                 + jnp.arange(SEQ, dtype=jnp.int32)[None, :])
    dsc = D_MODEL ** -0.5
    return {
        "x": x,
        "p": p,
        "positions": positions,
        "ln_mix": gain((DEPTH, D_MODEL)),
        "ln_ffn": gain((DEPTH, D_MODEL)),
        "ln_ple": gain((DEPTH, D_MODEL)),
        "a_w_in": nrm((N_EVEN, D_MODEL, EVEN_IN_WIDTH), dsc),
        "a_lambda": nrm((N_EVEN, 4, DA_HEAD_DIM), 0.1),
        "a_subln": gain((N_EVEN, DA_V_DIM)),
        "a_conv_w": nrm((N_EVEN, SC_WIDTH, SC_WIDTH_CONV), SC_WIDTH_CONV ** -0.5),
        "a_w_out": nrm((N_EVEN, DA_WIDTH + SC_WIDTH, D_MODEL), (DA_WIDTH + SC_WIDTH) ** -0.5),
        "ffn_w1": nrm((N_EVEN, D_MODEL, D_FF), dsc),
        "ffn_w3": nrm((N_EVEN, D_MODEL, D_FF), dsc),
        "ffn_w2": nrm((N_EVEN, D_FF, D_MODEL), D_FF ** -0.5),
        "c_w_in": nrm((N_ODD, D_MODEL, 2 * GM_WIDTH), dsc),
        "c_ln_g": gain((N_ODD, GM_WIDTH)),
        "c_ln_b": nrm((N_ODD, GM_WIDTH), 0.02),
        "c_w_s": nrm((N_ODD, GM_GROUPS, GM_CHUNK, GM_CHUNK), GM_CHUNK ** -0.5),
        "c_b_s": 1.0 + nrm((N_ODD, GM_GROUPS, GM_CHUNK), 0.1),
        "c_w_out": nrm((N_ODD, GM_WIDTH, D_MODEL), GM_WIDTH ** -0.5),
        "router_w": nrm((N_ODD, D_MODEL, N_EXPERTS), dsc),
        "moe_w1": nrm((N_ODD, N_EXPERTS, D_MODEL, D_FF), dsc),
        "moe_w3": nrm((N_ODD, N_EXPERTS, D_MODEL, D_FF), dsc),
        "moe_w2": nrm((N_ODD, N_EXPERTS, D_FF, D_MODEL), D_FF ** -0.5),
        "ple_gate": nrm((DEPTH, D_MODEL, D_MODEL), dsc),
        "ple_proj": nrm((DEPTH, PLE_DIM, D_MODEL), PLE_DIM ** -0.5),
        "final_norm": gain((D_MODEL,)),
    }


def reference(x, p, positions, ln_mix, ln_ffn, ln_ple, a_w_in, a_lambda, a_subln, a_conv_w,
              a_w_out, ffn_w1, ffn_w3, ffn_w2, c_w_in, c_ln_g, c_ln_b, c_w_s, c_b_s, c_w_out,
              router_w, moe_w1, moe_w3, moe_w2, ple_gate, ple_proj, final_norm):
    h = x
    bsz, seq, _ = x.shape
    splits = [DA_QK_WIDTH, 2 * DA_QK_WIDTH, 2 * DA_QK_WIDTH + DA_WIDTH,
              2 * DA_QK_WIDTH + DA_WIDTH + SC_WIDTH, 2 * DA_QK_WIDTH + DA_WIDTH + 2 * SC_WIDTH]
    for i in range(DEPTH):
        j = i // 2
        hn = rms_norm(h, ln_mix[i])
        if i % 2 == 0:
            z = hn @ a_w_in[j]
            q, k, v, b_gate, c_gate, hc = jnp.split(z, splits, axis=-1)
            q = rope_partial(q.reshape(bsz, seq, DA_HEADS, 2, DA_HEAD_DIM), positions)
            k = rope_partial(k.reshape(bsz, seq, DA_HEADS, 2, DA_HEAD_DIM), positions)
            v = v.reshape(bsz, seq, DA_HEADS, DA_V_DIM)
            lambda_init = 0.8 - 0.6 * math.exp(-0.3 * i)
            lp = a_lambda[j].astype(jnp.float32)
            lam = jnp.exp(jnp.sum(lp[0] * lp[1])) - jnp.exp(jnp.sum(lp[2] * lp[3])) + lambda_init
            attn = diff_attention(q, k, v, lam, lambda_init, a_subln[j])
            conv = b_gate * causal_short_conv(c_gate * hc, a_conv_w[j])
            h = h + jnp.concatenate([attn, conv], axis=-1) @ a_w_out[j]
            h = h + swiglu(rms_norm(h, ln_ffn[i]), ffn_w1[j], ffn_w3[j], ffn_w2[j])
        else:
            z = jax.nn.gelu(hn @ c_w_in[j], approximate=False)
            u, vv = jnp.split(z, 2, axis=-1)
            vv = layer_norm(vv, c_ln_g[j], c_ln_b[j])
            h = h + spatial_gating(u, vv, c_w_s[j], c_b_s[j]) @ c_w_out[j]
            h = h + moe_swiglu(rms_norm(h, ln_ffn[i]), router_w[j], moe_w1[j], moe_w3[j], moe_w2[j])
        gate = jax.nn.sigmoid(rms_norm(h, ln_ple[i]) @ ple_gate[i])
        h = h + gate * (p[i] @ ple_proj[i])
    return rms_norm(h, final_norm)
```

```python
import math
import numpy as np
from contextlib import ExitStack
import concourse.bass as bass
import concourse.mybir as mybir
from concourse.bass_utils import run_bass_kernel_spmd

F32 = mybir.dt.float32
BF16 = mybir.dt.bfloat16
I32 = mybir.dt.int32
ALU = mybir.AluOpType
AF = mybir.ActivationFunctionType
AX = mybir.AxisListType

EPOCH = 12000
NDMA_SEM = 8
COMPUTE = ("pe", "act", "dve", "pool")
QUEUES = ("sp", "pool")

NT = 2048
D = 1024
KC = 8
DFF = 2816
NF = 22
NE = 8
ARENA_WORDS = 53200


class Sched:
    def __init__(self):
        self.ops = []
        self.last_w = {}
        self.readers = {}
        self.pending = {}
        self.dma_since = []
        self.last_on = {}

    def op(self, eng, fn, reads=(), writes=(), dma=False):
        deps = set()
        for k in reads:
            w = self.last_w.get(k)
            if w is not None:
                deps.add(w)
        for k in writes:
            w = self.last_w.get(k)
            if w is not None:
                deps.add(w)
            rd = self.readers.get(k)
            if rd:
                deps.update(rd[0].values())
                deps.update(rd[1])
        pb = self.pending.pop(eng, None)
        if pb:
            deps.update(pb)
        oid = len(self.ops)
        self.ops.append(dict(eng=eng, fn=fn, deps=deps, dma=dma, inc=False))
        ws = set(writes)
        for k in writes:
            self.last_w[k] = oid
            self.readers[k] = [{}, []]
        for k in reads:
            if k in ws:
                continue
            rd = self.readers.setdefault(k, [{}, []])
            if dma:
                rd[1].append(oid)
            else:
                rd[0][eng] = oid
        if dma:
            self.dma_since.append(oid)
        else:
            self.last_on[eng] = oid
        return oid

    def barrier(self):
        deps = set(self.last_on.values()) | set(self.dma_since)
        self.dma_since = []
        for e in COMPUTE + ("sp",):
            self.pending.setdefault(e, set()).update(deps)

    def emit(self, nc, block, es):
        ops = self.ops
        qcount = {q: 0 for q in QUEUES}
        for op in ops:
            if op["dma"]:
                q = op["eng"]
                i = qcount[q]
                qcount[q] += 1
                op["slot"] = i % NDMA_SEM
                op["dval"] = 16 * (i // NDMA_SEM + 1)
        for op in ops:
            for d in op["deps"]:
                dop = ops[d]
                if dop["dma"]:
                    continue
                if dop["eng"] == op["eng"] and op["eng"] == "pe" and not op["dma"]:
                    continue
                dop["inc"] = True
        cnt = {e: 0 for e in COMPUTE}
        for op in ops:
            if not op["dma"] and op["inc"]:
                cnt[op["eng"]] += 1
                op["cnt"] = cnt[op["eng"]]
        nep = {e: max(1, -(-cnt[e] // EPOCH)) for e in COMPUTE}
        sems = {e: [es.enter_context(nc.semaphore(f"s_{e}_{i}")) for i in range(nep[e])] for e in COMPUTE}
        dsems = {q: [es.enter_context(nc.semaphore(f"d_{q}_{i}")) for i in range(NDMA_SEM)] for q in QUEUES}
        self.stats = dict(cnt=dict(cnt), nops=len(ops), qcount=dict(qcount))

        def run_engine(ename, eng):
            waited = {}
            for op in ops:
                if op["eng"] != ename:
                    continue
                need = {}
                for d in op["deps"]:
                    dop = ops[d]
                    if dop["dma"]:
                        key = ("d", dop["eng"], dop["slot"])
                        val = dop["dval"]
                    else:
                        if not dop["inc"]:
                            continue
                        if dop["eng"] == ename and ename == "pe" and not op["dma"]:
                            continue
                        key = ("c", dop["eng"])
                        val = dop["cnt"]
                    if val > need.get(key, 0):
                        need[key] = val
                if op["dma"] and op["dval"] > 16:
                    key = ("d", ename, op["slot"])
                    val = op["dval"] - 16
                    if val > need.get(key, 0):
                        need[key] = val
                for key, val in need.items():
                    if waited.get(key, 0) >= val:
                        continue
                    waited[key] = val
                    if key[0] == "d":
                        eng.wait_ge(dsems[key[1]][key[2]], val)
                    else:
                        ep = (val - 1) // EPOCH
                        eng.wait_ge(sems[key[1]][ep], (val - 1) % EPOCH + 1)
                ins = op["fn"](eng)
                if op["dma"]:
                    ins.then_inc(dsems[ename][op["slot"]], 16)
                elif op["inc"]:
                    ep = (op["cnt"] - 1) // EPOCH
                    ins.then_inc(sems[ename][ep], 1)
            if ename in QUEUES:
                n = qcount[ename]
                for s in range(NDMA_SEM):
                    uses = (n - s + NDMA_SEM - 1) // NDMA_SEM if n > s else 0
                    if uses > 0 and waited.get(("d", ename, s), 0) < 16 * uses:
                        eng.wait_ge(dsems[ename][s], 16 * uses)

        @block.sync
        def _(e):
            run_engine("sp", e)

        @block.tensor
        def _(e):
            run_engine("pe", e)

        @block.scalar
        def _(e):
            run_engine("act", e)

        @block.vector
        def _(e):
            run_engine("dve", e)

        @block.gpsimd
        def _(e):
            run_engine("pool", e)


C_ID, C_TRI, C_INVF, C_SGN, C_PMASK, C_ZERO, C_EPS, C_LNEPS, C_ONE, C_QTR, C_PIDX, C_F128, C_LS, NCONST = 0, 128, 256, 257, 258, 259, 260, 261, 262, 263, 264, 265, 287, 415

W_SPECS = [
    ("ln_mix", [2, 1024]), ("ln_ffn", [2, 1024]), ("ln_ple", [2, 1024]),
    ("a_w_in", [1024, 3072]), ("a_lambda", [256]), ("a_subln", [128]), ("a_conv_w", [512, 3]),
    ("a_w_out", [1024, 1024]), ("ffn_w1", [1024, DFF]), ("ffn_w3", [1024, DFF]), ("ffn_w2", [DFF, 1024]),
    ("c_w_in", [1024, 2048]), ("c_ln_g", [1024]), ("c_ln_b", [1024]), ("c_w_s", [8, 128, 128]),
    ("c_b_s", [1024]), ("c_w_out", [1024, 1024]), ("router_w", [1024, 8]),
    ("moe_w13", [8 * 22 * 128, 2048]), ("moe_w2", [8 * 11 * 128, 2048]),
    ("ple_gate", [2, 1024, 1024]), ("ple_proj", [2, 256, 1024]), ("final_norm", [1024]),
]


def build(stop=None):
    nc = bass.Bass("TRN2", target_bir_lowering=False)
    dr = {}
    dr["x_own"] = nc.dram_tensor("x_own", [NT, D], F32, kind="ExternalInput").ap()
    dr["x_pre"] = nc.dram_tensor("x_pre", [NT, D], F32, kind="ExternalInput").ap()
    dr["pos_own"] = nc.dram_tensor("pos_own", [NT], I32, kind="ExternalInput").ap()
    dr["pos_pre"] = nc.dram_tensor("pos_pre", [NT], I32, kind="ExternalInput").ap()
    dr["p_own"] = nc.dram_tensor("p_own", [2, NT, 256], F32, kind="ExternalInput").ap()
    dr["consts"] = nc.dram_tensor("consts", [128, NCONST], F32, kind="ExternalInput").ap()
    for name, shp in W_SPECS:
        dr[name] = nc.dram_tensor(name, shp, F32, kind="ExternalInput").ap()
    out_d = nc.dram_tensor("out", [NT, D], F32, kind="ExternalOutput").ap()

    es = ExitStack()
    with es:
        arena = es.enter_context(nc.sbuf_tensor("arena", [128, ARENA_WORDS], F32))
        pspair = [es.enter_context(nc.psum_tensor(f"pp{i}", [128, 1024], F32)) for i in range(4)]
        S = Sched()
        top = [0]

        def carve(n, dt=F32):
            words = n if dt in (F32, I32) else (n + 1) // 2
            a = arena[:, top[0]:top[0] + words]
            top[0] += words
            assert top[0] <= ARENA_WORDS, top[0]
            if dt != F32:
                a = a.bitcast(dt)
                if a.shape[1] != n:
                    a = a[:, 0:n]
            return a

        def DMA(q, out, in_, reads, writes):
            S.op(q, lambda e: e.dma_start(out=out, in_=in_), reads, writes, dma=True)

        def ACT(out, in_, func, reads, writes, **kw):
            S.op("act", lambda e: e.activation(out=out, in_=in_, func=func, **kw), reads, writes)

        def TT(eng, out, in0, in1, op, reads, writes):
            S.op(eng, lambda e: e.tensor_tensor(out=out, in0=in0, in1=in1, op=op), reads, writes)

        def TS(eng, out, in0, s1, s2, op0, op1, reads, writes):
            if s2 is None:
                S.op(eng, lambda e: e.tensor_single_scalar(out=out, in_=in0, scalar=s1, op=op0), reads, writes)
            else:
                S.op(eng, lambda e: e.tensor_scalar(out=out, in0=in0, scalar1=s1, scalar2=s2, op0=op0, op1=op1), reads, writes)

        def STT(eng, out, in0, scalar, in1, op0, op1, reads, writes):
            eng = "dve"
            S.op(eng, lambda e: e.scalar_tensor_tensor(out=out, in0=in0, scalar=scalar, in1=in1, op0=op0, op1=op1), reads, writes)

        def CP(eng, out, in_, reads, writes):
            if eng == "act":
                S.op("act", lambda e: e.copy(out=out, in_=in_), reads, writes)
            else:
                S.op(eng, lambda e: e.tensor_copy(out=out, in_=in_), reads, writes)

        def MM(ps, lhsT, rhs, start, stop, reads, writes, skip=False):
            if skip:
                S.op("pe", lambda e: e.matmul(ps, lhsT=lhsT, rhs=rhs, start=start, stop=stop, skip_group_check=True), reads, writes)
            else:
                S.op("pe", lambda e: e.matmul(ps, lhsT=lhsT, rhs=rhs, start=start, stop=stop), reads, writes)

        def TR(ps, in_, ident, reads, writes):
            S.op("pe", lambda e: e.transpose(out=ps, in_=in_, identity=ident), reads, writes)

        def RECIP(out, in_, reads, writes):
            S.op("dve", lambda e: e.reciprocal(out=out, in_=in_), reads, writes)

        def MEMSET(eng, ap, val, writes):
            S.op(eng, lambda e: e.memset(ap, val), (), writes)

        bank_rr = [0]

        def nbank(lo=0, hi=4):
            b = lo + bank_rr[0] % (hi - lo)
            bank_rr[0] += 1
            return b

        def PS(b):
            return pspair[b // 2][:, (b % 2) * 512:(b % 2 + 1) * 512]

        def PSB(b):
            return pspair[b // 2][:, (b % 2) * 512:(b % 2 + 1) * 512].bitcast(BF16)

        def pk(b):
            return ("ps", b)

        cst = carve(NCONST)
        identf = cst[:, C_ID:C_ID + 128]
        identb = carve(128, BF16)
        trib = carve(128, BF16)
        onesb = carve(128, BF16)
        gcols = carve(56)
        vrows = carve(128)
        convw = carve(12)
        neglam = carve(1)
        subg = carve(128)
        small = carve(64)
        col = lambda c: cst[:, c:c + 1]

        DMA("sp", cst, dr["consts"], [], ["cst"])
        CP("dve", identb, identf, ["cst"], ["identb"])
        CP("dve", trib, cst[:, C_TRI:C_TRI + 128], ["cst"], ["trib"])

        MEMSET("pool", onesb, 1.0, ["onesb"])
        MEMSET("pool", vrows, 0.0, ["vrows"])
        DMA("sp", vrows[0:16, :], dr["ln_mix"].rearrange("l (k p) -> (l k) p", p=128), ["vrows"], ["vrows"])
        DMA("sp", vrows[16:32, :], dr["ln_ffn"].rearrange("l (k p) -> (l k) p", p=128), ["vrows"], ["vrows"])
        DMA("sp", vrows[32:48, :], dr["ln_ple"].rearrange("l (k p) -> (l k) p", p=128), ["vrows"], ["vrows"])
        DMA("sp", vrows[48:56, :], dr["final_norm"].rearrange("(k p) -> k p", p=128), ["vrows"], ["vrows"])
        TR(PS(0)[:, 0:128], vrows, identf, ["vrows", "cst"], [pk(0)])
        CP("dve", gcols, PS(0)[:, 0:56], [pk(0)], ["gcols"])
        DMA("sp", convw.rearrange("p (c k) -> p c k", k=3), dr["a_conv_w"].rearrange("(c p) k -> p c k", p=128), [], ["convw"])
        DMA("sp", subg, dr["a_subln"].partition_broadcast(128), [], ["subg"])
        lambda_init0 = 0.8 - 0.6 * math.exp(0.0)
        TS("dve", subg, subg, 1.0 - lambda_init0, None, ALU.mult, None, ["subg"], ["subg"])
        lamb = carve(256)
        DMA("sp", lamb, dr["a_lambda"].partition_broadcast(128), [], ["lamb"])
        l4 = lamb.rearrange("p (a b d) -> p a b d", a=2, b=2)
        TT("dve", l4[:, :, 0, :], l4[:, :, 0, :], l4[:, :, 1, :], ALU.mult, ["lamb"], ["lamb"])
        S.op("dve", lambda e: e.reduce_sum(out=small[:, 0:2], in_=l4[:, :, 0, :], axis=AX.X), ["lamb"], ["small"])
        ACT(small[:, 2:4], small[:, 0:2], AF.Exp, ["small"], ["small"])
        TT("dve", small[:, 4:5], small[:, 3:4], small[:, 2:3], ALU.subtract, ["small"], ["small"])
        TS("dve", neglam, small[:, 4:5], -lambda_init0, None, ALU.add, None, ["small"], ["neglam"])
        top_persist = top[0]

        HT_WORDS = 8192 + 8320
        region = carve(HT_WORDS)
        hT = region[:, 0:16384].rearrange("p (k t) -> p k t", k=KC)
        k_rot = region[:, 0:8192].bitcast(BF16).rearrange("p (h t) -> p h t", h=4)
        Vp = region[:, 8192:16512].bitcast(BF16).rearrange("p (kb h d) -> p kb h d", kb=32, h=4)
        hnT = carve(KC * 1024, BF16).rearrange("p (k t) -> p k t", k=KC)
        top_phase = top[0]

        def hk(dm, tt):
            return ("hT", dm, tt)

        def load_w_piece(dst, src_ap, key):
            DMA("pool", dst, src_ap.rearrange("(k p) n -> p k n", p=128), [], [key])

        def fm_norm(gi, tt0, ntt, scr_sq, scr_r, want_f32=None):
            for i in range(ntt):
                tt = tt0 + i
                tsl = slice(tt * 512, (tt + 1) * 512)
                osl = slice(i * 512, (i + 1) * 512)
                hkeys = [hk(dm, tt) for dm in range(KC)]
                ACT(scr_sq, hT[:, :, tsl], AF.Square, hkeys, ["scr_sq"])
                b = nbank()
                for kc in range(KC):
                    MM(PS(b), onesb, scr_sq[:, kc, :], kc == 0, kc == KC - 1, ["scr_sq", "onesb"], [pk(b)])
                ACT(scr_r, PS(b), AF.Sqrt, [pk(b)], ["scr_r"], scale=1.0 / D, bias=col(C_EPS))
                RECIP(scr_r, scr_r, ["scr_r"], ["scr_r"])
                for kc in range(KC):
                    eng = "dve" if kc % 2 == 0 else "pool"
                    STT(eng, hnT[:, kc, osl], hT[:, kc, tsl], gcols[:, gi * 8 + kc:gi * 8 + kc + 1], scr_r,
                        ALU.mult, ALU.mult, [hk(kc, tt), "gcols", "scr_r"], [("hnT", i, kc)])
                    if want_f32 is not None:
                        STT(eng, want_f32[:, kc, osl], hT[:, kc, tsl], gcols[:, gi * 8 + kc:gi * 8 + kc + 1], scr_r,
                            ALU.mult, ALU.mult, [hk(kc, tt), "gcols", "scr_r"], [("hnF", i, kc)])

        q_rot = carve(4 * NT, BF16).rearrange("p (h t) -> p h t", h=4)
        gconvT = carve(4 * NT, BF16).rearrange("p (c t) -> p c t", c=4)
        wsl = [carve(KC * 128, BF16).rearrange("p (k n) -> p k n", k=KC) for _ in range(6)]
        l0_wtop = top[0]
        Wv = carve(KC * 512, BF16).rearrange("p (k n) -> p k n", k=KC)
        wpart = [carve(KC * 128, BF16).rearrange("p (k n) -> p k n", k=KC) for _ in range(2)]
        xt = [carve(D) for _ in range(2)]
        hn_tm = [carve(D, BF16) for _ in range(2)]
        gmix_bc = carve(D)
        posi = carve(512, I32)
        rtmp = [carve(512) for _ in range(3)]
        tabC = [carve(512) for _ in range(2)]
        tabS = [carve(512) for _ in range(2)]
        rt_base = top[0]
        rt1 = [carve(512) for _ in range(2)]
        rt2 = [carve(512) for _ in range(2)]
        ubuf = [carve(514) for _ in range(4)]
        csb = [carve(512) for _ in range(2)]
        ybuf = [carve(512) for _ in range(2)]
        PT = [carve(1024, BF16).rearrange("p (c n) -> p c n", c=2) for _ in range(3)]
        sqjunk = PT[2].rearrange("p c n -> p (c n)")
        obuf = [carve(128) for _ in range(2)]
        obuf2 = [carve(128) for _ in range(2)]
        onb = [carve(128, BF16) for _ in range(2)]
        l0_top = top[0]

        DMA("sp", gmix_bc, dr["ln_mix"][0].partition_broadcast(128), [], ["gmix_bc"])
        load_w_piece(Wv, dr["a_w_in"][:, 1024:1536], "Wv")
        MEMSET("pool", Vp[:, :, :, 128:130], 1.0, ["Vp_ones"])
        for cc in range(4):
            MEMSET("pool", ubuf[cc][:, 0:2], 0.0, [("u", cc)])

        piece_cols = []
        for ph_ in range(4):
            if True:
                for h_ in range(4):
                    piece_cols.append(512 + h_ * 128)
            if ph_ == 1:
                for cc_ in range(4):
                    piece_cols += [2048 + cc_ * 128, 2560 + cc_ * 128]
            if ph_ >= 2:
                for h_ in range(4):
                    piece_cols.append(h_ * 128)
                for cc_ in range(4):
                    piece_cols += [1536 + cc_ * 128, 2048 + cc_ * 128, 2560 + cc_ * 128]
        pc_issued = [0]
        pc_taken = [0]

        def take_piece(col):
            i = pc_taken[0]
            assert piece_cols[i] == col, (i, piece_cols[i], col)
            while pc_issued[0] < min(i + 3, len(piece_cols)):
                n = pc_issued[0]
                load_w_piece(wsl[n % 6], dr["a_w_in"][:, piece_cols[n]:piece_cols[n] + 128], ("wsl", n % 6))
                pc_issued[0] += 1
            pc_taken[0] += 1
            return i % 6

        wrr = [0]

        def wslot():
            i = wrr[0] % 6
            wrr[0] += 1
            return i

        def tm_norm_half(xsrc, t0):
            for ti in range(8):
                bi = ti % 2
                DMA("sp", xt[bi], xsrc[t0 + ti * 128:t0 + (ti + 1) * 128, :], [], [("xt", bi)])
                ACT(sqjunk, xt[bi], AF.Square, [("xt", bi)], ["sqjunk", ("ss", bi)], accum_out=small[:, 8 + bi:9 + bi])
                ACT(small[:, 10 + bi:11 + bi], small[:, 8 + bi:9 + bi], AF.Sqrt, [("ss", bi)], [("rs", bi)],
                    scale=1.0 / D, bias=col(C_EPS))
                RECIP(small[:, 10 + bi:11 + bi], small[:, 10 + bi:11 + bi], [("rs", bi)], [("rs", bi)])
                STT("dve", hn_tm[bi], xt[bi], small[:, 10 + bi:11 + bi], gmix_bc, ALU.mult, ALU.mult,
                    [("xt", bi), ("rs", bi), "gmix_bc"], [("hn_tm", bi)])
                b = nbank()
                for kc in range(KC):
                    TR(PSB(b)[:, kc * 128:(kc + 1) * 128], hn_tm[bi][:, kc * 128:(kc + 1) * 128], identb,
                       [("hn_tm", bi), "identb"], [pk(b)])
                CP("act", hnT[:, :, ti * 128:(ti + 1) * 128], PSB(b).rearrange("p (k t) -> p k t", k=KC),
                   [pk(b)], [("hnT", ti // 4, kc) for kc in range(KC)])

        def rope_tables(posv, t0, i):
            DMA("sp", posi, posv[t0:t0 + 512].partition_broadcast(128), [], ["posi"])
            y, yf, m = rtmp
            CP("dve", y, posi, ["posi"], ["rt_y"])
            TS("dve", y, y, col(C_INVF), None, ALU.mult, None, ["rt_y", "cst"], ["rt_y"])
            for which in range(2):
                if which == 1:
                    TS("dve", y, y, col(C_QTR), None, ALU.add, None, ["rt_y", "cst"], ["rt_y"])
                CP("dve", posi, y, ["rt_y"], ["posi"])
                CP("dve", yf, posi, ["posi"], ["rt_yf"])
                TT("dve", yf, y, yf, ALU.subtract, ["rt_y", "rt_yf"], ["rt_yf"])
                TS("dve", m, yf, 0.5, None, ALU.is_gt, None, ["rt_yf"], ["rt_m"])
                TT("dve", yf, yf, m, ALU.subtract, ["rt_yf", "rt_m"], ["rt_yf"])
                TS("dve", m, yf, -0.5, None, ALU.is_lt, None, ["rt_yf"], ["rt_m"])
                TT("dve", yf, yf, m, ALU.add, ["rt_yf", "rt_m"], ["rt_yf"])
                if which == 0:
                    ACT(tabS[i], yf, AF.Sin, ["rt_yf", "cst"], [("tabS", i)], scale=col(C_SGN))
                else:
                    ACT(tabC[i], yf, AF.Sin, ["rt_yf"], [("tabC", i)], scale=2.0 * math.pi)

        def make_partner(dst, src, skey, dkey):
            s4 = src.rearrange("p k (g j) -> p k g j", j=64)
            d4 = dst.rearrange("p k (g j) -> p k g j", j=64)
            CP("act", d4[:, :, :, 16:64], s4[:, :, :, 16:64], [skey], [dkey])
            CP("act", d4[:, :, :, 0:8], s4[:, :, :, 8:16], [skey], [dkey])
            CP("act", d4[:, :, :, 8:16], s4[:, :, :, 0:8], [skey], [dkey])

        def rope_prep(col0, h):
            si = take_piece(col0 + h * 128)
            make_partner(wpart[h % 2], wsl[si], ("wsl", si), ("wpart", h % 2))
            return si

        def rope_proj(si, h, ntile, dst_fn):
            pi = h % 2
            for i in range(ntile):
                tsl = slice(i * 512, (i + 1) * 512)
                ba, bb = nbank(), nbank()
                for kc in range(KC):
                    MM(PS(ba), wsl[si][:, kc, :], hnT[:, kc, tsl], kc == 0, kc == KC - 1,
                       [("wsl", si), ("hnT", i, kc)], [pk(ba)])
                for kc in range(KC):
                    MM(PS(bb), wpart[pi][:, kc, :], hnT[:, kc, tsl], kc == 0, kc == KC - 1,
                       [("wpart", pi), ("hnT", i, kc)], [pk(bb)])
                TT("dve", rt1[i], PS(ba), tabC[i], ALU.mult, [pk(ba), ("tabC", i)], [("rt1", i)])
                TT("dve", rt2[i], PS(bb), tabS[i], ALU.mult, [pk(bb), ("tabS", i)], [("rt2", i)])
                dap, dkey = dst_fn(i)
                TT("dve", dap, rt1[i], rt2[i], ALU.add, [("rt1", i), ("rt2", i)], [dkey])

        def rope_heads(col0, dst_of):
            si_next = rope_prep(col0, 0)
            for h in range(4):
                si = si_next
                if h + 1 < 4:
                    si_next = rope_prep(col0, h + 1)
                rope_proj(si, h, 2, dst_of(h))

        def v_proj(kb0):
            for ti in range(8):
                b = nbank()
                for kc in range(KC):
                    MM(PS(b), hnT[:, kc, ti * 128:(ti + 1) * 128], Wv[:, kc, :], kc == 0, kc == KC - 1,
                       [("hnT", ti // 4, kc), "Wv"], [pk(b)])
                CP("act", Vp[:, kb0 + ti, :, 0:128], PS(b).rearrange("p (h d) -> p h d", h=4), [pk(b)], [("Vp", kb0 + ti)])

        def conv_w_piece(which, cc):
            c0 = 1536 + which * 512 + cc * 128
            return take_piece(c0)

        for ph in range(4):
            is_pre = ph < 2
            hh = ph % 2
            xsrc = dr["x_pre"] if is_pre else dr["x_own"]
            posv = dr["pos_pre"] if is_pre else dr["pos_own"]
            t0 = hh * 1024
            kv0 = (0 if is_pre else NT) + t0
            def next_tables(ph=ph):
                if ph + 1 < 4:
                    nposv = dr["pos_pre"] if ph + 1 < 2 else dr["pos_own"]
                    nt0 = ((ph + 1) % 2) * 1024
                    for i_ in range(2):
                        rope_tables(nposv, nt0 + i_ * 512, i_)

            if ph == 0:
                for i in range(2):
                    rope_tables(posv, t0 + i * 512, i)
            tm_norm_half(xsrc, t0)
            rope_heads(512, lambda h: (lambda i, h=h: (k_rot[:, h, kv0 + i * 512:kv0 + (i + 1) * 512], ("k_rot", h, (kv0 // 512) + i))))
            if is_pre:
                next_tables()
            v_proj(kv0 // 128)
            if is_pre and hh == 1:
                for cc in range(4):
                    sc = conv_w_piece(1, cc)
                    sh = conv_w_piece(2, cc)
                    bc_, bh_ = nbank(), nbank()
                    for kc in range(KC):
                        MM(PS(bc_)[:, 0:2], wsl[sc][:, kc, :], hnT[:, kc, 1022:1024], kc == 0, kc == KC - 1,
                           [("wsl", sc), ("hnT", 1, kc)], [pk(bc_)])
                    for kc in range(KC):
                        MM(PS(bh_)[:, 0:2], wsl[sh][:, kc, :], hnT[:, kc, 1022:1024], kc == 0, kc == KC - 1,
                           [("wsl", sh), ("hnT", 1, kc)], [pk(bh_)])
                    CP("act", csb[0][:, 0:2], PS(bc_)[:, 0:2], [pk(bc_)], [("csb", 0)])
                    TT("dve", ubuf[cc][:, 0:2], PS(bh_)[:, 0:2], csb[0][:, 0:2], ALU.mult, [pk(bh_), ("csb", 0)], [("u", cc)])
            if not is_pre:
                rope_heads(0, lambda h: (lambda i, h=h: (q_rot[:, h, t0 + i * 512:t0 + (i + 1) * 512], ("q_rot", h, hh * 2 + i))))
                next_tables()
                for cc in range(4):
                    sb_ = conv_w_piece(0, cc)
                    sc = conv_w_piece(1, cc)
                    sh = conv_w_piece(2, cc)
                    for i in range(2):
                        tsl = slice(i * 512, (i + 1) * 512)
                        gt = hh * 2 + i
                        bb_, bc_, bh_ = nbank(), nbank(), nbank()
                        for (bk, sl_) in ((bc_, sc), (bh_, sh), (bb_, sb_)):
                            for kc in range(KC):
                                MM(PS(bk), wsl[sl_][:, kc, :], hnT[:, kc, tsl], kc == 0, kc == KC - 1,
                                   [("wsl", sl_), ("hnT", i, kc)], [pk(bk)])
                        j = i % 2
                        CP("act", csb[j], PS(bc_), [pk(bc_)], [("csb", j)])
                        u = ubuf[cc]
                        TT("dve", u[:, 2:514], PS(bh_), csb[j], ALU.mult, [pk(bh_), ("csb", j)], [("u", cc)])
                        y = ybuf[j]
                        cw = convw.rearrange("p (c k) -> p c k", k=3)
                        S.op("act", lambda e, y=y, u=u, cw=cw, cc=cc: e.mul(out=y, in_=u[:, 2:514], mul=cw[:, cc, 2:3]), [("u", cc), "convw"], [("y", j)])
                        STT("pool", y, u[:, 1:513], cw[:, cc, 1:2], y, ALU.mult, ALU.add, [("u", cc), "convw", ("y", j)], [("y", j)])
                        STT("pool", y, u[:, 0:512], cw[:, cc, 0:1], y, ALU.mult, ALU.add, [("u", cc), "convw", ("y", j)], [("y", j)])
                        TT("dve", gconvT[:, cc, gt * 512:(gt + 1) * 512], PS(bb_), y, ALU.mult, [pk(bb_), ("y", j)], [("gconvT", cc, gt)])
                        CP("pool", small[:, 16 + 2 * cc:18 + 2 * cc], u[:, 512:514], [("u", cc)], [("uh", cc)])
                        CP("pool", u[:, 0:2], small[:, 16 + 2 * cc:18 + 2 * cc], [("uh", cc)], [("u", cc)])

        acc = [None, None]
        for c in range(2):
            acc[c] = [PS(4 + 2 * c + j // 2)[:, (j % 2) * 256:(j % 2) * 256 + 129] for j in range(4)]
        S.barrier()
        accS = [arena[:, rt_base + c * 1024:rt_base + (c + 1) * 1024].rearrange("p (j d) -> p j d", j=4) for c in range(2)]
        steps = []
        for h in range(4):
            for qt in range(4):
                nown = 4 * qt + 4
                kbs = list(range(16)) + [16 + k for k in range(nown)]
                for ki, kb in enumerate(kbs):
                    steps.append(dict(h=h, qt=qt, kb=kb, first=(ki == 0), lastkb=(ki == len(kbs) - 1)))

        def st_geom(sp):
            is_own = sp["kb"] >= 16
            ko = sp["kb"] - 16
            j0 = max(0, ko - 4 * sp["qt"]) if is_own else 0
            return is_own, ko, j0, 512 - 128 * j0, sp["qt"] * 512 + 128 * j0

        def emit_st(k):
            sp = steps[k]
            is_own, ko, j0, ncol, q0 = st_geom(sp)
            h, kb = sp["h"], sp["kb"]
            pb = k % 2
            for c in range(2):
                psl = slice(64 * c, 64 * c + 64)
                MM(PS(2 * pb + c)[:, 0:ncol], k_rot[psl, h, kb * 128:(kb + 1) * 128], q_rot[psl, h, q0:q0 + ncol], True, True,
                   [("k_rot", h, kb // 4), ("q_rot", h, sp["qt"])], [pk(2 * pb + c)])

        def epilogue(h, qt):
            for c in range(2):
                for hb in range(2):
                    bz = 4 + 2 * c + hb
                    eng = "act" if hb == 0 else "dve"
                    CP(eng, accS[c][:, 2 * hb:2 * hb + 2, :].rearrange("p j d -> p (j d)"), PS(bz), [pk(bz)], [("accS", c, hb)])
            for j in range(4):
                oi = j % 2
                ka = [("accS", 0, j // 2), ("accS", 1, j // 2)]
                sc_ = small[:, 32 + 4 * oi:36 + 4 * oi]
                sk = ("sc", oi)
                RECIP(sc_[:, 0:1], accS[0][:, j, 128:129], ka, [sk])
                RECIP(sc_[:, 1:2], accS[1][:, j, 128:129], ka, [sk])
                TT("dve", sc_[:, 1:2], sc_[:, 1:2], neglam, ALU.mult, [sk, "neglam"], [sk])
                TS("dve", obuf[oi], accS[0][:, j, 0:128], sc_[:, 0:1], None, ALU.mult, None, ka + [sk], [("ob", oi)])
                STT("dve", obuf2[oi], accS[1][:, j, 0:128], sc_[:, 1:2], obuf[oi], ALU.mult, ALU.add, ka + [sk, ("ob", oi)], [("ob2", oi)])
                TT("dve", obuf[oi], obuf2[oi], obuf2[oi], ALU.mult, [("ob2", oi)], [("ob", oi)])
                S.op("dve", lambda e, oi=oi, sc_=sc_: e.reduce_sum(out=sc_[:, 2:3], in_=obuf[oi], axis=AX.X), [("ob", oi)], [sk])
                ACT(sc_[:, 3:4], sc_[:, 2:3], AF.Ln, [sk, "cst"], [sk], scale=1.0 / 128, bias=col(C_EPS))
                ACT(sc_[:, 3:4], sc_[:, 3:4], AF.Exp, [sk], [sk], scale=-0.5)
                STT("dve", onb[oi], obuf2[oi], sc_[:, 3:4], subg, ALU.mult, ALU.mult, [("ob2", oi), sk, "subg"], [("onb", oi)])
                TR(PSB(3)[:, 0:128], onb[oi], identb, [("onb", oi), "identb"], [pk(3)])
                CP("dve", q_rot[:, h, qt * 512 + j * 128:qt * 512 + (j + 1) * 128], PSB(3)[:, 0:128], [pk(3)], [("q_rot", h, qt)])

        emit_st(0)
        emit_st(1)
        for k, sp in enumerate(steps):
            is_own, ko, j0, ncol, q0 = st_geom(sp)
            h, qt, kb = sp["h"], sp["qt"], sp["kb"]
            pb = k % 2
            pi_ = k % 3
            if sp["first"]:
                for bz in (4, 5, 6, 7):
                    MEMSET("dve", PS(bz), 0.0, [pk(bz)])
            stv = pspair[pb][:, :].rearrange("p (c n) -> p c n", c=2)
            ACT(PT[pi_][:, :, 0:ncol], stv[:, :, 0:ncol], AF.Exp, [pk(2 * pb), pk(2 * pb + 1), "cst"], [("PT", pi_)],
                scale=0.125, bias=(col(C_ZERO) if is_own else col(C_PMASK)))
            if is_own and ko >= 4 * qt:
                for c in range(2):
                    TT("dve", PT[pi_][:, c, 0:128], PT[pi_][:, c, 0:128], trib, ALU.mult, [("PT", pi_), "trib"], [("PT", pi_)])
            if k + 2 < len(steps):
                emit_st(k + 2)
            for c in range(2):
                for j in range(j0, 4):
                    last = (kb == 16 + 4 * qt + j)
                    bkey = pk(4 + 2 * c + j // 2)
                    MM(acc[c][j], PT[pi_][:, c, (j - j0) * 128:(j - j0 + 1) * 128], Vp[:, kb, h, 0:129], False, last,
                       [("PT", pi_), ("Vp", kb), "Vp_ones"], [bkey], skip=True)
            if sp["lastkb"]:
                epilogue(h, qt)

        S.barrier()
        attnT = q_rot
        top[0] = l0_wtop
        xrow = [carve(D) for _ in range(4)]
        for tt in range(4):
            for r in range(4):
                DMA("sp", xrow[r], dr["x_own"][tt * 512 + r * 128:tt * 512 + (r + 1) * 128, :], [], [("xrow", r)])
            for dm in range(KC):
                si = wslot()
                load_w_piece(wsl[si], dr["a_w_out"][:, dm * 128:(dm + 1) * 128], ("wsl", si))
                bx, bo = nbank(), nbank()
                for r in range(4):
                    TR(PS(bx)[:, r * 128:(r + 1) * 128], xrow[r][:, dm * 128:(dm + 1) * 128], identf, [("xrow", r), "cst"], [pk(bx)])
                CP("act", hT[:, dm, tt * 512:(tt + 1) * 512], PS(bx), [pk(bx)], [hk(dm, tt)])
                for kc in range(KC):
                    rhs = attnT[:, kc, tt * 512:(tt + 1) * 512] if kc < 4 else gconvT[:, kc - 4, tt * 512:(tt + 1) * 512]
                    rk = ("q_rot", kc, tt) if kc < 4 else ("gconvT", kc - 4, tt)
                    MM(PS(bo), wsl[si][:, kc, :], rhs, kc == 0, kc == KC - 1, [("wsl", si), rk], [pk(bo)])
                TT("dve", hT[:, dm, tt * 512:(tt + 1) * 512], PS(bo), hT[:, dm, tt * 512:(tt + 1) * 512], ALU.add,
                   [pk(bo), hk(dm, tt)], [hk(dm, tt)])
        S.barrier()
        top[0] = top_phase

        scr_sq = carve(KC * 512, BF16).rearrange("p (k t) -> p k t", k=KC)
        scr_r = carve(512)
        w13_base = top[0]
        w13 = [carve(KC * 128, BF16).rearrange("p (k n) -> p k n", k=KC) for _ in range(6)]
        w2s_base = top[0]
        w2s = [carve(NF * 128, BF16).rearrange("p (f n) -> p f n", f=NF) for _ in range(2)]
        sil = [carve(512) for _ in range(2)]
        gen_top = top[0]
        gT_region = carve(NF * 512)
        gT = gT_region.bitcast(BF16).rearrange("p (f t) -> p f t", f=NF)
        after_gT = top[0]
        top[0] = gen_top
        pT = carve(2 * 1024, BF16).rearrange("p (k t) -> p k t", k=2)
        prow = [carve(256) for _ in range(2)]
        prow_b = [carve(256, BF16) for _ in range(2)]
        sg = [carve(512) for _ in range(2)]
        w13rr = [0]
        w2rr = [0]

        def swiglu_group(w1, w3, w2, g0, post):
            def load_w2(dm):
                s2 = (w2rr[0] + dm) % 2
                DMA("pool", w2s[s2], w2[:, dm * 128:(dm + 1) * 128].rearrange("(f p) n -> p f n", p=128), [], [("w2s", s2)])

            for f in range(NF):
                s1 = w13rr[0] % 6
                s3 = (w13rr[0] + 1) % 6
                w13rr[0] += 2
                load_w_piece(w13[s1], w1[:, f * 128:(f + 1) * 128], ("w13", s1))
                load_w_piece(w13[s3], w3[:, f * 128:(f + 1) * 128], ("w13", s3))
                if f == NF - 4:
                    load_w2(0)
                if f == NF - 2:
                    load_w2(1)
                for i in range(2):
                    tsl = slice(i * 512, (i + 1) * 512)
                    ba, bb = nbank(0, 6), nbank(0, 6)
                    for kc in range(KC):
                        MM(PS(ba), w13[s1][:, kc, :], hnT[:, kc, tsl], kc == 0, kc == KC - 1, [("w13", s1), ("hnT", i, kc)], [pk(ba)])
                    for kc in range(KC):
                        MM(PS(bb), w13[s3][:, kc, :], hnT[:, kc, tsl], kc == 0, kc == KC - 1, [("w13", s3), ("hnT", i, kc)], [pk(bb)])
                    ACT(sil[i], PS(ba), AF.Silu, [pk(ba)], [("sil", i)])
                    TT("dve", gT[:, f, tsl], PS(bb), sil[i], ALU.mult, [pk(bb), ("sil", i)], [("gT", f, i)])
            for dm in range(KC):
                s2 = (w2rr[0] + dm) % 2
                for i in range(2):
                    tsl = slice(i * 512, (i + 1) * 512)
                    b = nbank(6, 8)
                    for f in range(NF):
                        MM(PS(b), w2s[s2][:, f, :], gT[:, f, tsl], f == 0, f == NF - 1, [("w2s", s2), ("gT", f, i)], [pk(b)])
                    post(b, dm, g0 * 2 + i)
                if dm + 2 < KC:
                    load_w2(dm + 2)

        def add_to_h(b, dm, tt):
            TT("dve", hT[:, dm, tt * 512:(tt + 1) * 512], PS(b), hT[:, dm, tt * 512:(tt + 1) * 512], ALU.add,
               [pk(b), hk(dm, tt)], [hk(dm, tt)])

        def ple(layer):
            gi = 4 + layer
            for g in range(2):
                for ti in range(8):
                    bi = ti % 2
                    DMA("sp", prow[bi], dr["p_own"][layer, g * 1024 + ti * 128:g * 1024 + (ti + 1) * 128, :], [], [("prow", bi)])
                    CP("dve", prow_b[bi], prow[bi], [("prow", bi)], [("prow_b", bi)])
                    b = nbank(0, 6)
                    for k2 in range(2):
                        TR(PSB(b)[:, k2 * 128:(k2 + 1) * 128], prow_b[bi][:, k2 * 128:(k2 + 1) * 128], identb, [("prow_b", bi), "identb"], [pk(b)])
                    CP("act", pT[:, :, ti * 128:(ti + 1) * 128], PSB(b)[:, 0:256].rearrange("p (k t) -> p k t", k=2), [pk(b)], [("pT", ti // 4)])
                fm_norm(gi, g * 2, 2, scr_sq, scr_r)
                for dm in range(KC):
                    sgw = w13rr[0] % 6
                    spw = (w13rr[0] + 1) % 6
                    w13rr[0] += 2
                    load_w_piece(w13[sgw], dr["ple_gate"][layer][:, dm * 128:(dm + 1) * 128], ("w13", sgw))
                    load_w_piece(w13[spw][:, 0:2, :], dr["ple_proj"][layer][:, dm * 128:(dm + 1) * 128], ("w13", spw))
                    for i in range(2):
                        tsl = slice(i * 512, (i + 1) * 512)
                        tt = g * 2 + i
                        bg, bp = nbank(0, 6), nbank(0, 6)
                        for kc in range(KC):
                            MM(PS(bg), w13[sgw][:, kc, :], hnT[:, kc, tsl], kc == 0, kc == KC - 1, [("w13", sgw), ("hnT", i, kc)], [pk(bg)])
                        for k2 in range(2):
                            MM(PS(bp), w13[spw][:, k2, :], pT[:, k2, tsl], k2 == 0, k2 == 1, [("w13", spw), ("pT", i)], [pk(bp)])
                        ACT(sg[i], PS(bg), AF.Sigmoid, [pk(bg)], [("sg", i)])
                        TT("dve", sg[i], PS(bp), sg[i], ALU.mult, [pk(bp), ("sg", i)], [("sg", i)])
                        TT("dve", hT[:, dm, tt * 512:(tt + 1) * 512], hT[:, dm, tt * 512:(tt + 1) * 512], sg[i], ALU.add,
                           [("sg", i), hk(dm, tt)], [hk(dm, tt)])

        def dump_h():
            pass

        if stop != "l0mix":
            for g in range(2):
                fm_norm(2, g * 2, 2, scr_sq, scr_r)
                swiglu_group(dr["ffn_w1"], dr["ffn_w3"], dr["ffn_w2"], g, add_to_h)
            S.barrier()
        if stop not in ("l0mix", "l0ffn"):
            ple(0)
            S.barrier()

        if stop not in ("l0mix", "l0ffn", "l0ple"):
            top[0] = gen_top
            uT = carve(KC * 1024, BF16).rearrange("p (k t) -> p k t", k=KC)
            suT = uT
            vvn = carve(8 * 1024, BF16).rearrange("p (i n) -> p i n", i=8)
            wsT = carve(8 * 128, BF16).rearrange("p (g t) -> p g t", g=8)
            wsf = carve(128)
            wsb = carve(128, BF16)
            bs_bc = carve(1024)
            lng_bc = carve(1024)
            lnb_bc = carve(1024)
            wcv = [carve(KC * 512, BF16).rearrange("p (k n) -> p k n", k=KC) for _ in range(2)]
            vraw = [carve(1024) for _ in range(2)]
            vtmp = [carve(1024) for _ in range(2)]
            vjunk = carve(1024, BF16)
            vst = [carve(8) for _ in range(2)]
            vsbig = vtmp
            DMA("sp", bs_bc, dr["c_b_s"].partition_broadcast(128), [], ["bs_bc"])
            DMA("sp", lng_bc, dr["c_ln_g"].partition_broadcast(128), [], ["lng_bc"])
            DMA("sp", lnb_bc, dr["c_ln_b"].partition_broadcast(128), [], ["lnb_bc"])
            for g8 in range(8):
                DMA("sp", wsf, dr["c_w_s"][g8], ["wsf"], ["wsf"])
                CP("dve", wsb, wsf, ["wsf"], ["wsb"])
                TR(PSB(0)[:, 0:128], wsb, identb, ["wsb", "identb"], [pk(0)])
                TT("dve", wsT[:, g8, :], PSB(0)[:, 0:128], trib, ALU.mult, [pk(0), "trib"], ["wsT"])
            for g in range(2):
                fm_norm(1, g * 2, 2, scr_sq, scr_r)
                for fc in range(KC):
                    s1 = w13rr[0] % 6
                    w13rr[0] += 1
                    load_w_piece(w13[s1], dr["c_w_in"][:, fc * 128:(fc + 1) * 128], ("w13", s1))
                    for i in range(2):
                        tsl = slice(i * 512, (i + 1) * 512)
                        b = nbank(0, 6)
                        for kc in range(KC):
                            MM(PS(b), w13[s1][:, kc, :], hnT[:, kc, tsl], kc == 0, kc == KC - 1, [("w13", s1), ("hnT", i, kc)], [pk(b)])
                        ACT(uT[:, fc, tsl], PS(b), AF.Gelu, [pk(b)], [("uT", fc, i)])
                for hv in range(2):
                    load_w_piece(wcv[hv], dr["c_w_in"][:, 1024 + hv * 512:1024 + (hv + 1) * 512], ("wcv", hv))
                for ti in range(8):
                    bi = ti % 2
                    for hv in range(2):
                        b = nbank(0, 6)
                        for kc in range(KC):
                            MM(PS(b), hnT[:, kc, ti * 128:(ti + 1) * 128], wcv[hv][:, kc, :], kc == 0, kc == KC - 1,
                               [("hnT", ti // 4, kc), ("wcv", hv)], [pk(b)])
                        ACT(vraw[bi][:, hv * 512:(hv + 1) * 512], PS(b), AF.Gelu, [pk(b)], [("vraw", bi)])
                    st = vst[bi]
                    sk = ("vst", bi)
                    S.op("dve", lambda e, bi=bi, st=st: e.reduce_sum(out=st[:, 0:1], in_=vraw[bi], axis=AX.X), [("vraw", bi)], [sk])
                    TS("dve", st[:, 1:2], st[:, 0:1], -1.0 / 1024, None, ALU.mult, None, [sk], [sk])
                    S.op("act", lambda e, bi=bi, st=st: e.add(out=vtmp[bi], in_=vraw[bi], add=st[:, 1:2]), [("vraw", bi), sk], [("vtmp", bi)])
                    ACT(vjunk, vtmp[bi], AF.Square, [("vtmp", bi)], ["vjunk", sk], accum_out=st[:, 2:3])
                    ACT(st[:, 3:4], st[:, 2:3], AF.Sqrt, [sk, "cst"], [sk], scale=1.0 / 1024, bias=col(C_LNEPS))
                    RECIP(st[:, 3:4], st[:, 3:4], [sk], [sk])
                    STT("dve", vtmp[bi], vtmp[bi], st[:, 3:4], lng_bc, ALU.mult, ALU.mult, [("vtmp", bi), sk, "lng_bc"], [("vtmp", bi)])
                    TT("dve", vvn[:, ti, :], vtmp[bi], lnb_bc, ALU.add, [("vtmp", bi), "lnb_bc"], [("vvn", ti)])
                for ti in range(8):
                    pp_ = ti % 2
                    i = ti // 4
                    for g8 in range(8):
                        bk = 2 * pp_ + g8 // 4
                        MM(PS(bk)[:, (g8 % 4) * 128:(g8 % 4 + 1) * 128], vvn[:, ti, g8 * 128:(g8 + 1) * 128], wsT[:, g8, :], True, True,
                           [("vvn", ti), "wsT"], [pk(bk)])
                    vb = vsbig[pp_]
                    TT("dve", vb, pspair[pp_][:, :], bs_bc, ALU.add, [pk(2 * pp_), pk(2 * pp_ + 1), "bs_bc"], [("vtmp", pp_)])
                    uv = uT[:, :, ti * 128:(ti + 1) * 128]
                    TT("dve", uv, vb.rearrange("p (g t) -> p g t", g=8), uv, ALU.mult,
                       [("vtmp", pp_)] + [("uT", g8, i) for g8 in range(8)], [("uT", g8, i) for g8 in range(8)])
                for dm in range(KC):
                    s1 = w13rr[0] % 6
                    w13rr[0] += 1
                    load_w_piece(w13[s1], dr["c_w_out"][:, dm * 128:(dm + 1) * 128], ("w13", s1))
                    for i in range(2):
                        tsl = slice(i * 512, (i + 1) * 512)
                        b = nbank(6, 8)
                        for kc in range(KC):
                            MM(PS(b), w13[s1][:, kc, :], suT[:, kc, tsl], kc == 0, kc == KC - 1, [("w13", s1), ("uT", kc, i)], [pk(b)])
                        add_to_h(b, dm, g * 2 + i)
            S.barrier()

        if stop not in ("l0mix", "l0ffn", "l0ple", "l1mix"):
            top[0] = gen_top
            NB = 15
            M1all = carve(128).rearrange("p (i e) -> p i e", e=8)
            M2all = carve(128).rearrange("p (i e) -> p i e", e=8)
            Call = carve(128).rearrange("p (i e) -> p i e", e=8)
            G1all = carve(16)
            G2all = carve(16)
            S1f = carve(16)
            S2f = carve(16)
            S1i = carve(16, I32)
            S2i = carve(16, I32)
            Msum = carve(8)
            Mi = [carve(8) for _ in range(2)]
            cntf = carve(8)
            cnti = carve(8, I32)
            padf = carve(8)
            psf = carve(8)
            pef = carve(8)
            ebf = carve(16)
            basef = carve(16)
            tq = [carve(8) for _ in range(2)]
            onesf = vrows
            rw = carve(KC * 8).rearrange("p (k e) -> p k e", k=KC)
            idxf = carve(33)
            idxi = [carve(33, I32) for _ in range(2)]
            base2f = carve(16)
            moe_base = top[0]
            hn_d = None
            xs_d = nc.dram_tensor("xs_scr", [NB * 512, D], BF16, kind="Internal").ap()
            ys_d = nc.dram_tensor("ys_scr", [NB * 512, D], F32, kind="Internal").ap()
            hnF = carve(KC * 1024).rearrange("p (k t) -> p k t", k=KC)
            hn_all = carve(16 * 1024, BF16).rearrange("p (i n) -> p i n", i=16)
            lg = [carve(8) for _ in range(2)]
            l2 = [carve(8) for _ in range(2)]
            rsm = [carve(8) for _ in range(2)]
            MEMSET("dve", Msum, 0.0, ["Msum"])
            MEMSET("dve", onesf, 1.0, ["onesf"])
            zt = carve(1024, BF16)
            MEMSET("dve", zt, 0.0, ["zt"])
            for zi_ in range(NB * 4):
                DMA("sp", xs_d[zi_ * 128:(zi_ + 1) * 128, :], zt, ["zt"], [("xsz", zi_)])
            xsz_keys = [("xsz", zi_) for zi_ in range(NB * 4)]
            DMA("sp", rw, dr["router_w"].rearrange("(k p) e -> p k e", p=128), [], ["rw"])
            LS = cst[:, C_LS:C_LS + 128]
            for g in range(2):
                fm_norm(3, g * 2, 2, scr_sq, scr_r, want_f32=hnF)
                for ti in range(8):
                    i = g * 8 + ti
                    bi = ti % 2
                    b = nbank(0, 6)
                    for kc in range(KC):
                        MM(PS(b)[:, 0:8], hnF[:, kc, ti * 128:(ti + 1) * 128], rw[:, kc, :], kc == 0, kc == KC - 1,
                           [("hnF", ti // 4, kc), "rw"], [pk(b)])
                    CP("dve", lg[bi], PS(b)[:, 0:8], [pk(b)], [("lg", bi)])
                    r = rsm[bi]
                    rk = ("rsm", bi)
                    mk1 = M1all[:, i, :]
                    mk2 = M2all[:, i, :]
                    S.op("dve", lambda e, r=r, bi=bi: e.reduce_max(out=r[:, 0:1], in_=lg[bi], axis=AX.X), [("lg", bi)], [rk])
                    TS("dve", mk1, lg[bi], r[:, 0:1], None, ALU.is_equal, None, [("lg", bi), rk], [("M1", i)])
                    STT("dve", l2[bi], mk1, -1e30, lg[bi], ALU.mult, ALU.add, [("M1", i), ("lg", bi)], [("l2", bi)])
                    S.op("dve", lambda e, r=r, bi=bi: e.reduce_max(out=r[:, 1:2], in_=l2[bi], axis=AX.X), [("l2", bi)], [rk])
                    TS("dve", mk2, l2[bi], r[:, 1:2], None, ALU.is_equal, None, [("l2", bi), rk], [("M2", i)])
                    TT("dve", r[:, 2:3], r[:, 0:1], r[:, 1:2], ALU.subtract, [rk], [rk])
                    ACT(G1all[:, i:i + 1], r[:, 2:3], AF.Sigmoid, [rk], [("G1", i)])
                    TS("dve", G2all[:, i:i + 1], G1all[:, i:i + 1], -1.0, 1.0, ALU.mult, ALU.add, [("G1", i)], [("G2", i)])
                    TT("dve", Mi[bi], mk1, mk2, ALU.add, [("M1", i), ("M2", i)], [("Mi", bi)])
                    b2 = nbank(0, 6)
                    MM(PS(b2)[:, 0:8], LS, Mi[bi], True, False, ["cst", ("Mi", bi)], [pk(b2)])
                    MM(PS(b2)[:, 0:8], onesf, Msum, False, True, ["onesf", "Msum"], [pk(b2)])
                    CP("dve", Call[:, i, :], PS(b2)[:, 0:8], [pk(b2)], [("Call", i)])
                    TT("dve", Msum, Msum, Mi[bi], ALU.add, ["Msum", ("Mi", bi)], ["Msum"])
                    b3 = nbank(0, 6)
                    for kc in range(KC):
                        TR(PSB(b3)[:, kc * 128:(kc + 1) * 128], hnT[:, kc, ti * 128:(ti + 1) * 128], identb,
                           [("hnT", ti // 4, kc), "identb"], [pk(b3)])
                    CP("act", hn_all[:, i, :], PSB(b3), [pk(b3)], [("hn_all", i)])
            b = nbank(0, 6)
            MM(PS(b)[:, 0:8], onesf, Msum, True, True, ["onesf", "Msum"], [pk(b)])
            CP("dve", cntf, PS(b)[:, 0:8], [pk(b)], ["cntf"])
            MEMSET("dve", padf, 0.0, ["padf"])
            for m_ in range(4):
                STT("dve", padf, cntf, 512.0 * m_, padf, ALU.is_gt, ALU.add, ["cntf", "padf"], ["padf"])
            TS("dve", padf, padf, 512.0, None, ALU.mult, None, ["padf"], ["padf"])
            MEMSET("dve", psf, 0.0, ["psf"])
            for e8 in range(1, NE):
                TT("dve", psf[:, e8:e8 + 1], psf[:, e8 - 1:e8], padf[:, e8 - 1:e8], ALU.add, ["psf", "padf"], ["psf"])
            TT("dve", pef, psf, padf, ALU.add, ["psf", "padf"], ["pef"])
            for i in range(16):
                bi = i % 2
                TT("dve", tq[bi], Call[:, i, :], psf, ALU.add, [("Call", i), "psf"], [("tq", bi)])
                TT("dve", lg[bi], tq[bi], M1all[:, i, :], ALU.mult, [("tq", bi), ("M1", i)], [("lg", bi)])
                S.op("dve", lambda e, i=i, bi=bi: e.reduce_sum(out=S1f[:, i:i + 1], in_=lg[bi], axis=AX.X), [("lg", bi)], ["S1f"])
                TT("dve", l2[bi], tq[bi], M2all[:, i, :], ALU.mult, [("tq", bi), ("M2", i)], [("l2", bi)])
                S.op("dve", lambda e, i=i, bi=bi: e.reduce_sum(out=S2f[:, i:i + 1], in_=l2[bi], axis=AX.X), [("l2", bi)], ["S2f"])
            CP("dve", S1i, S1f, ["S1f"], ["S1i"])
            CP("dve", S2i, S2f, ["S2f"], ["S2i"])
            MEMSET("dve", ebf, 0.0, ["ebf"])
            for bb in range(NB):
                bi = bb % 2
                TS("dve", tq[bi], pef, 512.0 * bb, None, ALU.is_le, None, ["pef"], [("tq", bi)])
                S.op("dve", lambda e, bb=bb, bi=bi: e.reduce_sum(out=ebf[:, bb:bb + 1], in_=tq[bi], axis=AX.X), [("tq", bi)], ["ebf"])
            TS("dve", ebf, ebf, 7.0, 2816.0, ALU.min, ALU.mult, ["ebf"], ["ebf"])
            TS("dve", basef, ebf, col(C_PIDX), None, ALU.add, None, ["ebf", "cst"], ["basef"])
            TS("dve", base2f, ebf, 0.5, None, ALU.mult, None, ["ebf"], ["base2f"])
            TS("dve", base2f, base2f, col(C_PIDX), None, ALU.add, None, ["base2f", "cst"], ["base2f"])
            for i in range(16):
                S.op("pool", lambda e, i=i: e.indirect_dma_start(out=xs_d, out_offset=bass.IndirectOffsetOnAxis(ap=S1i[:, i:i + 1], axis=0),
                                                                 in_=hn_all[:, i, :], in_offset=None),
                     [("hn_all", i), "S1i"] + xsz_keys, [("xs", i, 0)], dma=True)
                S.op("pool", lambda e, i=i: e.indirect_dma_start(out=xs_d, out_offset=bass.IndirectOffsetOnAxis(ap=S2i[:, i:i + 1], axis=0),
                                                                 in_=hn_all[:, i, :], in_offset=None),
                     [("hn_all", i), "S2i"] + xsz_keys, [("xs", i, 1)], dma=True)
            S.barrier()
            xs_keys = [("xs", i, w_) for i in range(16) for w_ in range(2)]
            top[0] = moe_base
            XsT = carve(KC * 512, BF16).rearrange("p (k t) -> p k t", k=KC)
            xs_tm = [carve(1024, BF16) for _ in range(4)]
            gTb = carve(NF * 512, BF16).rearrange("p (f t) -> p f t", f=NF)
            w13p = [arena[:, w13_base + k * 1024:w13_base + (k + 1) * 1024].bitcast(BF16).rearrange("p (w k n) -> p w k n", w=2, k=KC) for k in range(3)]
            w2p = [arena[:, w2s_base + k * 1024:w2s_base + (k + 1) * 1024].bitcast(BF16).rearrange("p (w d) -> p w d", w=2) for k in range(2)]
            w2p.append(carve(2048, BF16).rearrange("p (w d) -> p w d", w=2))
            stage = [carve(2048) for _ in range(3)]
            ystage = [carve(1024) for _ in range(2)]
            w13r = dr["moe_w13"]
            w2f = dr["moe_w2"]
            stg = [0]
            w13p_rr = [0]
            w2r_rr = [0]
            ys_rr = [0]

            def gather_piece(src, ii, colidx, dst_bf, dst_key, cast_eng):
                sl = stg[0] % 3
                stg[0] += 1
                S.op("pool", lambda e, sl=sl, ii=ii, colidx=colidx, src=src: e.indirect_dma_start(
                    out=stage[sl], out_offset=None, in_=src, in_offset=bass.IndirectOffsetOnAxis(ap=idxi[ii][:, colidx:colidx + 1], axis=0)),
                    [("idxi", ii)], [("stage", sl)], dma=True)
                CP(cast_eng, dst_bf, stage[sl], [("stage", sl)], [dst_key])

            XsT2 = [XsT, carve(KC * 512, BF16).rearrange("p (k t) -> p k t", k=KC)]

            def make_idx(bb):
                ii = bb % 2
                TS("dve", idxf[:, 0:22], cst[:, C_F128:C_F128 + 22], basef[:, bb:bb + 1], None, ALU.add, None, ["cst", "basef"], ["idxf"])
                TS("dve", idxf[:, 22:33], cst[:, C_F128:C_F128 + 11], base2f[:, bb:bb + 1], None, ALU.add, None, ["cst", "base2f"], ["idxf"])
                CP("dve", idxi[ii], idxf, ["idxf"], [("idxi", ii)])

            def load_xs(bb):
                for j in range(4):
                    DMA("sp", xs_tm[j], xs_d[bb * 512 + j * 128:bb * 512 + (j + 1) * 128, :], xs_keys, [("xs_tm", j)])

            def transpose_xs(bb):
                xt_ = XsT2[bb % 2]
                for j in range(4):
                    b3 = nbank(0, 6)
                    for kc in range(KC):
                        TR(PSB(b3)[:, kc * 128:(kc + 1) * 128], xs_tm[j][:, kc * 128:(kc + 1) * 128], identb, [("xs_tm", j), "identb"], [pk(b3)])
                    CP("dve", xt_[:, :, j * 128:(j + 1) * 128], PSB(b3).rearrange("p (k t) -> p k t", k=KC), [pk(b3)], [("XsT", bb % 2, j)])

            def fetch13(bb, f):
                sp_ = (bb * NF + f) % 3
                gather_piece(w13r, bb % 2, f, w13p[sp_].rearrange("p w k n -> p (w k n)"), ("w13p", sp_), "act")

            def fetch2(bb, fp):
                sr = (bb * 11 + fp) % 3
                gather_piece(w2f, bb % 2, 22 + fp, w2p[sr].rearrange("p w d -> p (w d)"), ("w2p", sr), "dve")

            make_idx(0)
            load_xs(0)
            transpose_xs(0)
            fetch13(0, 0)
            for bb in range(NB):
                xt_ = XsT2[bb % 2]
                xk = [("XsT", bb % 2, j) for j in range(4)]
                if bb + 1 < NB:
                    make_idx(bb + 1)
                bank_rr[0] = 0
                for f in range(NF):
                    sp_ = (bb * NF + f) % 3
                    if f + 1 < NF:
                        fetch13(bb, f + 1)
                    else:
                        fetch2(bb, 0)
                    if f == 8 and bb + 1 < NB:
                        load_xs(bb + 1)
                    ba, bb_ = nbank(0, 6), nbank(0, 6)
                    for kc in range(KC):
                        MM(PS(ba), w13p[sp_][:, 0, kc, :], xt_[:, kc, :], kc == 0, kc == KC - 1, [("w13p", sp_)] + xk, [pk(ba)])
                    for kc in range(KC):
                        MM(PS(bb_), w13p[sp_][:, 1, kc, :], xt_[:, kc, :], kc == 0, kc == KC - 1, [("w13p", sp_)] + xk, [pk(bb_)])
                    ACT(sil[f % 2], PS(ba), AF.Silu, [pk(ba)], [("sil", f % 2)])
                    TT("dve", gTb[:, f, :], PS(bb_), sil[f % 2], ALU.mult, [pk(bb_), ("sil", f % 2)], [("gTb", f)])
                if bb + 1 < NB:
                    transpose_xs(bb + 1)
                for fp in range(11):
                    sr = (bb * 11 + fp) % 3
                    if fp + 1 < 11:
                        fetch2(bb, fp + 1)
                    elif bb + 1 < NB:
                        fetch13(bb + 1, 0)
                    for two in range(2):
                        f = 2 * fp + two
                        for j in range(4):
                            for hf in range(2):
                                bk = j * 2 + hf
                                MM(PS(bk), gTb[:, f, j * 128:(j + 1) * 128], w2p[sr][:, two, hf * 512:(hf + 1) * 512], f == 0, f == NF - 1,
                                   [("gTb", f), ("w2p", sr)], [pk(bk)])
                for j in range(4):
                    yi = ys_rr[0] % 2
                    ys_rr[0] += 1
                    CP("act", ystage[yi][:, 0:512], PS(j * 2), [pk(j * 2)], [("ystage", yi)])
                    CP("dve", ystage[yi][:, 512:1024], PS(j * 2 + 1), [pk(j * 2 + 1)], [("ystage", yi)])
                    DMA("sp", ys_d[bb * 512 + j * 128:bb * 512 + (j + 1) * 128, :], ystage[yi], [("ystage", yi)], [("ys", bb, j)])
            S.barrier()
            ys_keys = [("ys", bb, j) for bb in range(NB) for j in range(4)]
            ypair = [(stage[0][:, 0:1024], stage[0][:, 1024:2048]), (stage[2][:, 0:1024], stage[2][:, 1024:2048])]
            zb = [stage[1][:, 0:1024], stage[1][:, 1024:2048]]
            for i in range(16):
                zi = i % 2
                y1, y2 = ypair[zi]
                S.op("pool", lambda e, i=i, y1=y1: e.indirect_dma_start(out=y1, out_offset=None, in_=ys_d, in_offset=bass.IndirectOffsetOnAxis(ap=S1i[:, i:i + 1], axis=0)),
                     ys_keys + ["S1i"], [("y1", zi)], dma=True)
                S.op("pool", lambda e, i=i, y2=y2: e.indirect_dma_start(out=y2, out_offset=None, in_=ys_d, in_offset=bass.IndirectOffsetOnAxis(ap=S2i[:, i:i + 1], axis=0)),
                     ys_keys + ["S2i"], [("y2", zi)], dma=True)
                TS("dve", zb[zi], y1, G1all[:, i:i + 1], None, ALU.mult, None, [("y1", zi), ("G1", i)], [("zb", zi)])
                STT("dve", zb[zi], y2, G2all[:, i:i + 1], zb[zi], ALU.mult, ALU.add, [("y2", zi), ("G2", i), ("zb", zi)], [("zb", zi)])
                for half in range(2):
                    b = nbank(0, 6)
                    for k4 in range(4):
                        kc = half * 4 + k4
                        TR(PS(b)[:, k4 * 128:(k4 + 1) * 128], zb[zi][:, kc * 128:(kc + 1) * 128], identf, [("zb", zi), "cst"], [pk(b)])
                    tt_ = i // 4
                    hv = hT[:, half * 4:(half + 1) * 4, i * 128:(i + 1) * 128]
                    TT("dve", hv, PS(b).rearrange("p (k t) -> p k t", k=4), hv, ALU.add,
                       [pk(b)] + [hk(half * 4 + k4, tt_) for k4 in range(4)], [hk(half * 4 + k4, tt_) for k4 in range(4)])
            S.barrier()

        if stop not in ("l0mix", "l0ffn", "l0ple", "l1mix", "l1moe"):
            ple(1)
            S.barrier()

        top[0] = after_gT
        fin = carve(KC * 512).rearrange("p (k t) -> p k t", k=KC)
        orow = [carve(D) for _ in range(2)]
        final = stop is None
        for tt in range(4):
            tsl = slice(tt * 512, (tt + 1) * 512)
            hkeys = [hk(dm, tt) for dm in range(KC)]
            if final:
                ACT(scr_sq, hT[:, :, tsl], AF.Square, hkeys, ["scr_sq"])
                b = nbank(0, 6)
                for kc in range(KC):
                    MM(PS(b), onesb, scr_sq[:, kc, :], kc == 0, kc == KC - 1, ["scr_sq", "onesb"], [pk(b)])
                ACT(scr_r, PS(b), AF.Sqrt, [pk(b)], ["scr_r"], scale=1.0 / D, bias=col(C_EPS))
                RECIP(scr_r, scr_r, ["scr_r"], ["scr_r"])
                for kc in range(KC):
                    eng = "dve" if kc % 2 == 0 else "pool"
                    STT(eng, fin[:, kc, :], hT[:, kc, tsl], gcols[:, 48 + kc:49 + kc], scr_r, ALU.mult, ALU.mult,
                        [hk(kc, tt), "gcols", "scr_r"], ["fin"])
                src = fin
                skeys = ["fin"]
            else:
                src = hT[:, :, tsl]
                skeys = hkeys
            for r in range(4):
                oi = (tt * 4 + r) % 2
                for half in range(2):
                    b = nbank(0, 6)
                    for k4 in range(4):
                        kc = half * 4 + k4
                        TR(PS(b)[:, k4 * 128:(k4 + 1) * 128], src[:, kc, r * 128:(r + 1) * 128], identf, skeys + ["cst"], [pk(b)])
                    CP("act" if half == 0 else "dve", orow[oi][:, half * 512:(half + 1) * 512], PS(b), [pk(b)], [("orow", oi)])
                DMA("sp", out_d[tt * 512 + r * 128:tt * 512 + (r + 1) * 128, :], orow[oi], [("orow", oi)], ["out"])

        block = es.enter_context(nc.Block())
        S.emit(nc, block, es)
        build.stats = S.stats
    return nc


def make_consts(half):
    c = np.zeros((128, NCONST), np.float32)
    c[:, C_ID:C_ID + 128] = np.eye(128, dtype=np.float32)
    p = np.arange(128)[:, None]
    f = np.arange(128)[None, :]
    c[:, C_TRI:C_TRI + 128] = (f >= p).astype(np.float32)
    rot_dim = 16
    inv_freq = (500000.0 ** (-np.arange(0, rot_dim, 2, dtype=np.float32) / rot_dim)).astype(np.float32)
    for pp in range(128):
        d = pp % 64
        if d < 16:
            c[pp, C_INVF] = inv_freq[d % 8] / (2.0 * math.pi)
            c[pp, C_SGN] = (-1.0 if d < 8 else 1.0) * 2.0 * math.pi
    c[:, C_PMASK] = 0.0 if half == 1 else -30000.0
    c[:, C_ZERO] = 0.0
    c[:, C_EPS] = 1e-6
    c[:, C_LNEPS] = 1e-5
    c[:, C_ONE] = 1.0
    c[:, C_QTR] = 0.25
    c[:, C_PIDX] = np.arange(128)
    c[:, C_F128:C_F128 + 22] = (np.arange(22) * 128)[None, :]
    c[:, C_LS:C_LS + 128] = (f > p).astype(np.float32)
    return c


def make_in_maps(inputs):
    x = np.asarray(inputs["x"], np.float32)
    p = np.asarray(inputs["p"], np.float32)
    pos = np.asarray(inputs["positions"], np.int32)
    shared = {}
    for name, shp in W_SPECS:
        if name == "moe_w13":
            w1t = np.asarray(inputs["moe_w1"], np.float32).reshape(8, 8, 128, 22, 128).transpose(0, 3, 2, 1, 4)
            w3t = np.asarray(inputs["moe_w3"], np.float32).reshape(8, 8, 128, 22, 128).transpose(0, 3, 2, 1, 4)
            a = np.stack([w1t, w3t], axis=3)
        elif name == "moe_w2":
            a = np.asarray(inputs[name], np.float32).reshape(8, 11, 2, 128, 1024).transpose(0, 1, 3, 2, 4)
        else:
            a = np.asarray(inputs[name], np.float32)
        shared[name] = np.ascontiguousarray(a.reshape(shp))
    in_maps = []
    for c in range(8):
        b, half = c // 2, c % 2
        sl = slice(half * NT, (half + 1) * NT)
        m = dict(shared)
        m["x_own"] = np.ascontiguousarray(x[b, sl])
        m["pos_own"] = np.ascontiguousarray(pos[b, sl])
        if half == 1:
            m["x_pre"] = np.ascontiguousarray(x[b, 0:NT])
            m["pos_pre"] = np.ascontiguousarray(pos[b, 0:NT])
        else:
            m["x_pre"] = np.zeros((NT, D), np.float32)
            m["pos_pre"] = np.zeros((NT,), np.int32)
        m["p_own"] = np.ascontiguousarray(p[:, b, sl])
        m["consts"] = make_consts(half)
        in_maps.append(m)
    return in_maps


def kernel(**inputs):
    nc = build()
    in_maps = make_in_maps(inputs)
    res = run_bass_kernel_spmd(nc, in_maps, core_ids=list(range(8)))
    out = np.zeros((4, 2 * NT, D), np.float32)
    for c in range(8):
        b, half = c // 2, c % 2
        out[b, half * NT:(half + 1) * NT] = res.results[c]["out"]
    return out
```

```python
import math
import numpy as np
from contextlib import ExitStack
import concourse.bass as bass
import concourse.mybir as mybir
from concourse.bass_utils import run_bass_kernel_spmd

F32 = mybir.dt.float32
BF16 = mybir.dt.bfloat16
I32 = mybir.dt.int32
ALU = mybir.AluOpType
AF = mybir.ActivationFunctionType
AX = mybir.AxisListType

EPOCH = 12000
NDMA_SEM = 8
COMPUTE = ("pe", "act", "dve", "pool")
QUEUES = ("sp", "pool")

NT = 2048
D = 1024
KC = 8
DFF = 2816
NF = 22
NE = 8
ARENA_WORDS = 53200


class Sched:
    def __init__(self):
        self.ops = []
        self.last_w = {}
        self.readers = {}
        self.pending = {}
        self.dma_since = []
        self.last_on = {}

    def op(self, eng, fn, reads=(), writes=(), dma=False):
        deps = set()
        for k in reads:
            w = self.last_w.get(k)
            if w is not None:
                deps.add(w)
        for k in writes:
            w = self.last_w.get(k)
            if w is not None:
                deps.add(w)
            rd = self.readers.get(k)
            if rd:
                deps.update(rd[0].values())
                deps.update(rd[1])
        pb = self.pending.pop(eng, None)
        if pb:
            deps.update(pb)
        oid = len(self.ops)
        self.ops.append(dict(eng=eng, fn=fn, deps=deps, dma=dma, inc=False))
        ws = set(writes)
        for k in writes:
            self.last_w[k] = oid
            self.readers[k] = [{}, []]
        for k in reads:
            if k in ws:
                continue
            rd = self.readers.setdefault(k, [{}, []])
            if dma:
                rd[1].append(oid)
            else:
                rd[0][eng] = oid
        if dma:
            self.dma_since.append(oid)
        else:
            self.last_on[eng] = oid
        return oid

    def barrier(self):
        deps = set(self.last_on.values()) | set(self.dma_since)
        self.dma_since = []
        for e in COMPUTE + ("sp",):
            self.pending.setdefault(e, set()).update(deps)

    def emit(self, nc, block, es):
        ops = self.ops
        qcount = {q: 0 for q in QUEUES}
        for op in ops:
            if op["dma"]:
                q = op["eng"]
                i = qcount[q]
                qcount[q] += 1
                op["slot"] = i % NDMA_SEM
                op["dval"] = 16 * (i // NDMA_SEM + 1)
        for op in ops:
            for d in op["deps"]:
                dop = ops[d]
                if dop["dma"]:
                    continue
                if dop["eng"] == op["eng"] and op["eng"] == "pe" and not op["dma"]:
                    continue
                dop["inc"] = True
        cnt = {e: 0 for e in COMPUTE}
        for op in ops:
            if not op["dma"] and op["inc"]:
                cnt[op["eng"]] += 1
                op["cnt"] = cnt[op["eng"]]
        nep = {e: max(1, -(-cnt[e] // EPOCH)) for e in COMPUTE}
        sems = {e: [es.enter_context(nc.semaphore(f"s_{e}_{i}")) for i in range(nep[e])] for e in COMPUTE}
        dsems = {q: [es.enter_context(nc.semaphore(f"d_{q}_{i}")) for i in range(NDMA_SEM)] for q in QUEUES}
        self.stats = dict(cnt=dict(cnt), nops=len(ops), qcount=dict(qcount))

        def run_engine(ename, eng):
            waited = {}
            for op in ops:
                if op["eng"] != ename:
                    continue
                need = {}
                for d in op["deps"]:
                    dop = ops[d]
                    if dop["dma"]:
                        key = ("d", dop["eng"], dop["slot"])
                        val = dop["dval"]
                    else:
                        if not dop["inc"]:
                            continue
                        if dop["eng"] == ename and ename == "pe" and not op["dma"]:
                            continue
                        key = ("c", dop["eng"])
                        val = dop["cnt"]
                    if val > need.get(key, 0):
                        need[key] = val
                if op["dma"] and op["dval"] > 16:
                    key = ("d", ename, op["slot"])
                    val = op["dval"] - 16
                    if val > need.get(key, 0):
                        need[key] = val
                for key, val in need.items():
                    if waited.get(key, 0) >= val:
                        continue
                    waited[key] = val
                    if key[0] == "d":
                        eng.wait_ge(dsems[key[1]][key[2]], val)
                    else:
                        ep = (val - 1) // EPOCH
                        eng.wait_ge(sems[key[1]][ep], (val - 1) % EPOCH + 1)
                ins = op["fn"](eng)
                if op["dma"]:
                    ins.then_inc(dsems[ename][op["slot"]], 16)
                elif op["inc"]:
                    ep = (op["cnt"] - 1) // EPOCH
                    ins.then_inc(sems[ename][ep], 1)
            if ename in QUEUES:
                n = qcount[ename]
                for s in range(NDMA_SEM):
                    uses = (n - s + NDMA_SEM - 1) // NDMA_SEM if n > s else 0
                    if uses > 0 and waited.get(("d", ename, s), 0) < 16 * uses:
                        eng.wait_ge(dsems[ename][s], 16 * uses)

        @block.sync
        def _(e):
            run_engine("sp", e)

        @block.tensor
        def _(e):
            run_engine("pe", e)

        @block.scalar
        def _(e):
            run_engine("act", e)

        @block.vector
        def _(e):
            run_engine("dve", e)

        @block.gpsimd
        def _(e):
            run_engine("pool", e)


C_ID, C_TRI, C_INVF, C_SGN, C_PMASK, C_ZERO, C_EPS, C_LNEPS, C_ONE, C_QTR, C_PIDX, C_F128, C_LS, NCONST = 0, 128, 256, 257, 258, 259, 260, 261, 262, 263, 264, 265, 287, 415

W_SPECS = [
    ("ln_mix", [2, 1024]), ("ln_ffn", [2, 1024]), ("ln_ple", [2, 1024]),
    ("a_w_in", [1024, 3072]), ("a_lambda", [256]), ("a_subln", [128]), ("a_conv_w", [512, 3]),
    ("a_w_out", [1024, 1024]), ("ffn_w1", [1024, DFF]), ("ffn_w3", [1024, DFF]), ("ffn_w2", [DFF, 1024]),
    ("c_w_in", [1024, 2048]), ("c_ln_g", [1024]), ("c_ln_b", [1024]), ("c_w_s", [8, 128, 128]),
    ("c_b_s", [1024]), ("c_w_out", [1024, 1024]), ("router_w", [1024, 8]),
    ("moe_w13", [8 * 22 * 128, 2048]), ("moe_w2", [8 * 11 * 128, 2048]),
    ("ple_gate", [2, 1024, 1024]), ("ple_proj", [2, 256, 1024]), ("final_norm", [1024]),
]


def build(stop=None):
    nc = bass.Bass("TRN2", target_bir_lowering=False)
    dr = {}
    dr["x_own"] = nc.dram_tensor("x_own", [NT, D], F32, kind="ExternalInput").ap()
    dr["x_pre"] = nc.dram_tensor("x_pre", [NT, D], F32, kind="ExternalInput").ap()
    dr["pos_own"] = nc.dram_tensor("pos_own", [NT], I32, kind="ExternalInput").ap()
    dr["pos_pre"] = nc.dram_tensor("pos_pre", [NT], I32, kind="ExternalInput").ap()
    dr["p_own"] = nc.dram_tensor("p_own", [2, NT, 256], F32, kind="ExternalInput").ap()
    dr["consts"] = nc.dram_tensor("consts", [128, NCONST], F32, kind="ExternalInput").ap()
    for name, shp in W_SPECS:
        dr[name] = nc.dram_tensor(name, shp, F32, kind="ExternalInput").ap()
    out_d = nc.dram_tensor("out", [NT, D], F32, kind="ExternalOutput").ap()

    es = ExitStack()
    with es:
        arena = es.enter_context(nc.sbuf_tensor("arena", [128, ARENA_WORDS], F32))
        pspair = [es.enter_context(nc.psum_tensor(f"pp{i}", [128, 1024], F32)) for i in range(4)]
        S = Sched()
        top = [0]

        def carve(n, dt=F32):
            words = n if dt in (F32, I32) else (n + 1) // 2
            a = arena[:, top[0]:top[0] + words]
            top[0] += words
            assert top[0] <= ARENA_WORDS, top[0]
            if dt != F32:
                a = a.bitcast(dt)
                if a.shape[1] != n:
                    a = a[:, 0:n]
            return a

        def DMA(q, out, in_, reads, writes):
            S.op(q, lambda e: e.dma_start(out=out, in_=in_), reads, writes, dma=True)

        def ACT(out, in_, func, reads, writes, **kw):
            S.op("act", lambda e: e.activation(out=out, in_=in_, func=func, **kw), reads, writes)

        def TT(eng, out, in0, in1, op, reads, writes):
            S.op(eng, lambda e: e.tensor_tensor(out=out, in0=in0, in1=in1, op=op), reads, writes)

        def TS(eng, out, in0, s1, s2, op0, op1, reads, writes):
            if s2 is None:
                S.op(eng, lambda e: e.tensor_single_scalar(out=out, in_=in0, scalar=s1, op=op0), reads, writes)
            else:
                S.op(eng, lambda e: e.tensor_scalar(out=out, in0=in0, scalar1=s1, scalar2=s2, op0=op0, op1=op1), reads, writes)

        def STT(eng, out, in0, scalar, in1, op0, op1, reads, writes):
            eng = "dve"
            S.op(eng, lambda e: e.scalar_tensor_tensor(out=out, in0=in0, scalar=scalar, in1=in1, op0=op0, op1=op1), reads, writes)

        def CP(eng, out, in_, reads, writes):
            if eng == "act":
                S.op("act", lambda e: e.copy(out=out, in_=in_), reads, writes)
            else:
                S.op(eng, lambda e: e.tensor_copy(out=out, in_=in_), reads, writes)

        def MM(ps, lhsT, rhs, start, stop, reads, writes, skip=False):
            if skip:
                S.op("pe", lambda e: e.matmul(ps, lhsT=lhsT, rhs=rhs, start=start, stop=stop, skip_group_check=True), reads, writes)
            else:
                S.op("pe", lambda e: e.matmul(ps, lhsT=lhsT, rhs=rhs, start=start, stop=stop), reads, writes)

        def TR(ps, in_, ident, reads, writes):
            S.op("pe", lambda e: e.transpose(out=ps, in_=in_, identity=ident), reads, writes)

        def RECIP(out, in_, reads, writes):
            S.op("dve", lambda e: e.reciprocal(out=out, in_=in_), reads, writes)

        def MEMSET(eng, ap, val, writes):
            S.op(eng, lambda e: e.memset(ap, val), (), writes)

        bank_rr = [0]

        def nbank(lo=0, hi=4):
            b = lo + bank_rr[0] % (hi - lo)
            bank_rr[0] += 1
            return b

        def PS(b):
            return pspair[b // 2][:, (b % 2) * 512:(b % 2 + 1) * 512]

        def PSB(b):
            return pspair[b // 2][:, (b % 2) * 512:(b % 2 + 1) * 512].bitcast(BF16)

        def pk(b):
            return ("ps", b)

        cst = carve(NCONST)
        identf = cst[:, C_ID:C_ID + 128]
        identb = carve(128, BF16)
        trib = carve(128, BF16)
        onesb = carve(128, BF16)
        gcols = carve(56)
        vrows = carve(128)
        convw = carve(12)
        neglam = carve(1)
        subg = carve(128)
        small = carve(64)
        col = lambda c: cst[:, c:c + 1]

        DMA("sp", cst, dr["consts"], [], ["cst"])
        CP("dve", identb, identf, ["cst"], ["identb"])
        CP("dve", trib, cst[:, C_TRI:C_TRI + 128], ["cst"], ["trib"])

        MEMSET("pool", onesb, 1.0, ["onesb"])
        MEMSET("pool", vrows, 0.0, ["vrows"])
        DMA("sp", vrows[0:16, :], dr["ln_mix"].rearrange("l (k p) -> (l k) p", p=128), ["vrows"], ["vrows"])
        DMA("sp", vrows[16:32, :], dr["ln_ffn"].rearrange("l (k p) -> (l k) p", p=128), ["vrows"], ["vrows"])
        DMA("sp", vrows[32:48, :], dr["ln_ple"].rearrange("l (k p) -> (l k) p", p=128), ["vrows"], ["vrows"])
        DMA("sp", vrows[48:56, :], dr["final_norm"].rearrange("(k p) -> k p", p=128), ["vrows"], ["vrows"])
        TR(PS(0)[:, 0:128], vrows, identf, ["vrows", "cst"], [pk(0)])
        CP("dve", gcols, PS(0)[:, 0:56], [pk(0)], ["gcols"])
        DMA("sp", convw.rearrange("p (c k) -> p c k", k=3), dr["a_conv_w"].rearrange("(c p) k -> p c k", p=128), [], ["convw"])
        DMA("sp", subg, dr["a_subln"].partition_broadcast(128), [], ["subg"])
        lambda_init0 = 0.8 - 0.6 * math.exp(0.0)
        TS("dve", subg, subg, 1.0 - lambda_init0, None, ALU.mult, None, ["subg"], ["subg"])
        lamb = carve(256)
        DMA("sp", lamb, dr["a_lambda"].partition_broadcast(128), [], ["lamb"])
        l4 = lamb.rearrange("p (a b d) -> p a b d", a=2, b=2)
        TT("dve", l4[:, :, 0, :], l4[:, :, 0, :], l4[:, :, 1, :], ALU.mult, ["lamb"], ["lamb"])
        S.op("dve", lambda e: e.reduce_sum(out=small[:, 0:2], in_=l4[:, :, 0, :], axis=AX.X), ["lamb"], ["small"])
        ACT(small[:, 2:4], small[:, 0:2], AF.Exp, ["small"], ["small"])
        TT("dve", small[:, 4:5], small[:, 3:4], small[:, 2:3], ALU.subtract, ["small"], ["small"])
        TS("dve", neglam, small[:, 4:5], -lambda_init0, None, ALU.add, None, ["small"], ["neglam"])
        top_persist = top[0]

        HT_WORDS = 8192 + 8320
        region = carve(HT_WORDS)
        hT = region[:, 0:16384].rearrange("p (k t) -> p k t", k=KC)
        k_rot = region[:, 0:8192].bitcast(BF16).rearrange("p (h t) -> p h t", h=4)
        Vp = region[:, 8192:16512].bitcast(BF16).rearrange("p (kb h d) -> p kb h d", kb=32, h=4)
        hnT = carve(KC * 1024, BF16).rearrange("p (k t) -> p k t", k=KC)
        top_phase = top[0]

        def hk(dm, tt):
            return ("hT", dm, tt)

        def load_w_piece(dst, src_ap, key):
            DMA("pool", dst, src_ap.rearrange("(k p) n -> p k n", p=128), [], [key])

        def fm_norm(gi, tt0, ntt, scr_sq, scr_r, want_f32=None):
            for i in range(ntt):
                tt = tt0 + i
                tsl = slice(tt * 512, (tt + 1) * 512)
                osl = slice(i * 512, (i + 1) * 512)
                hkeys = [hk(dm, tt) for dm in range(KC)]
                ACT(scr_sq, hT[:, :, tsl], AF.Square, hkeys, ["scr_sq"])
                b = nbank()
                for kc in range(KC):
                    MM(PS(b), onesb, scr_sq[:, kc, :], kc == 0, kc == KC - 1, ["scr_sq", "onesb"], [pk(b)])
                ACT(scr_r, PS(b), AF.Sqrt, [pk(b)], ["scr_r"], scale=1.0 / D, bias=col(C_EPS))
                RECIP(scr_r, scr_r, ["scr_r"], ["scr_r"])
                for kc in range(KC):
                    eng = "dve" if kc % 2 == 0 else "pool"
                    STT(eng, hnT[:, kc, osl], hT[:, kc, tsl], gcols[:, gi * 8 + kc:gi * 8 + kc + 1], scr_r,
                        ALU.mult, ALU.mult, [hk(kc, tt), "gcols", "scr_r"], [("hnT", i, kc)])
                    if want_f32 is not None:
                        STT(eng, want_f32[:, kc, osl], hT[:, kc, tsl], gcols[:, gi * 8 + kc:gi * 8 + kc + 1], scr_r,
                            ALU.mult, ALU.mult, [hk(kc, tt), "gcols", "scr_r"], [("hnF", i, kc)])

        q_rot = carve(4 * NT, BF16).rearrange("p (h t) -> p h t", h=4)
        gconvT = carve(4 * NT, BF16).rearrange("p (c t) -> p c t", c=4)
        wsl = [carve(KC * 128, BF16).rearrange("p (k n) -> p k n", k=KC) for _ in range(6)]
        l0_wtop = top[0]
        Wv = carve(KC * 512, BF16).rearrange("p (k n) -> p k n", k=KC)
        wpart = [carve(KC * 128, BF16).rearrange("p (k n) -> p k n", k=KC) for _ in range(2)]
        xt = [carve(D) for _ in range(2)]
        hn_tm = [carve(D, BF16) for _ in range(2)]
        gmix_bc = carve(D)
        posi = carve(512, I32)
        rtmp = [carve(512) for _ in range(3)]
        tabC = [carve(512) for _ in range(2)]
        tabS = [carve(512) for _ in range(2)]
        rt_base = top[0]
        rt1 = [carve(512) for _ in range(2)]
        rt2 = [carve(512) for _ in range(2)]
        ubuf = [carve(514) for _ in range(4)]
        csb = [carve(512) for _ in range(2)]
        ybuf = [carve(512) for _ in range(2)]
        PT = [carve(1024, BF16).rearrange("p (c n) -> p c n", c=2) for _ in range(3)]
        sqjunk = PT[2].rearrange("p c n -> p (c n)")
        obuf = [carve(128) for _ in range(2)]
        obuf2 = [carve(128) for _ in range(2)]
        onb = [carve(128, BF16) for _ in range(2)]
        l0_top = top[0]

        DMA("sp", gmix_bc, dr["ln_mix"][0].partition_broadcast(128), [], ["gmix_bc"])
        load_w_piece(Wv, dr["a_w_in"][:, 1024:1536], "Wv")
        MEMSET("pool", Vp[:, :, :, 128:130], 1.0, ["Vp_ones"])
        for cc in range(4):
            MEMSET("pool", ubuf[cc][:, 0:2], 0.0, [("u", cc)])

        piece_cols = []
        for ph_ in range(4):
            if True:
                for h_ in range(4):
                    piece_cols.append(512 + h_ * 128)
            if ph_ == 1:
                for cc_ in range(4):
                    piece_cols += [2048 + cc_ * 128, 2560 + cc_ * 128]
            if ph_ >= 2:
                for h_ in range(4):
                    piece_cols.append(h_ * 128)
                for cc_ in range(4):
                    piece_cols += [1536 + cc_ * 128, 2048 + cc_ * 128, 2560 + cc_ * 128]
        pc_issued = [0]
        pc_taken = [0]

        def take_piece(col):
            i = pc_taken[0]
            assert piece_cols[i] == col, (i, piece_cols[i], col)
            while pc_issued[0] < min(i + 3, len(piece_cols)):
                n = pc_issued[0]
                load_w_piece(wsl[n % 6], dr["a_w_in"][:, piece_cols[n]:piece_cols[n] + 128], ("wsl", n % 6))
                pc_issued[0] += 1
            pc_taken[0] += 1
            return i % 6

        wrr = [0]

        def wslot():
            i = wrr[0] % 6
            wrr[0] += 1
            return i

        def tm_norm_half(xsrc, t0):
            for ti in range(8):
                bi = ti % 2
                DMA("sp", xt[bi], xsrc[t0 + ti * 128:t0 + (ti + 1) * 128, :], [], [("xt", bi)])
                ACT(sqjunk, xt[bi], AF.Square, [("xt", bi)], ["sqjunk", ("ss", bi)], accum_out=small[:, 8 + bi:9 + bi])
                ACT(small[:, 10 + bi:11 + bi], small[:, 8 + bi:9 + bi], AF.Sqrt, [("ss", bi)], [("rs", bi)],
                    scale=1.0 / D, bias=col(C_EPS))
                RECIP(small[:, 10 + bi:11 + bi], small[:, 10 + bi:11 + bi], [("rs", bi)], [("rs", bi)])
                STT("dve", hn_tm[bi], xt[bi], small[:, 10 + bi:11 + bi], gmix_bc, ALU.mult, ALU.mult,
                    [("xt", bi), ("rs", bi), "gmix_bc"], [("hn_tm", bi)])
                b = nbank()
                for kc in range(KC):
                    TR(PSB(b)[:, kc * 128:(kc + 1) * 128], hn_tm[bi][:, kc * 128:(kc + 1) * 128], identb,
                       [("hn_tm", bi), "identb"], [pk(b)])
                CP("act", hnT[:, :, ti * 128:(ti + 1) * 128], PSB(b).rearrange("p (k t) -> p k t", k=KC),
                   [pk(b)], [("hnT", ti // 4, kc) for kc in range(KC)])

        def rope_tables(posv, t0, i):
            DMA("sp", posi, posv[t0:t0 + 512].partition_broadcast(128), [], ["posi"])
            y, yf, m = rtmp
            CP("dve", y, posi, ["posi"], ["rt_y"])
            TS("dve", y, y, col(C_INVF), None, ALU.mult, None, ["rt_y", "cst"], ["rt_y"])
            for which in range(2):
                if which == 1:
                    TS("dve", y, y, col(C_QTR), None, ALU.add, None, ["rt_y", "cst"], ["rt_y"])
                CP("dve", posi, y, ["rt_y"], ["posi"])
                CP("dve", yf, posi, ["posi"], ["rt_yf"])
                TT("dve", yf, y, yf, ALU.subtract, ["rt_y", "rt_yf"], ["rt_yf"])
                TS("dve", m, yf, 0.5, None, ALU.is_gt, None, ["rt_yf"], ["rt_m"])
                TT("dve", yf, yf, m, ALU.subtract, ["rt_yf", "rt_m"], ["rt_yf"])
                TS("dve", m, yf, -0.5, None, ALU.is_lt, None, ["rt_yf"], ["rt_m"])
                TT("dve", yf, yf, m, ALU.add, ["rt_yf", "rt_m"], ["rt_yf"])
                if which == 0:
                    ACT(tabS[i], yf, AF.Sin, ["rt_yf", "cst"], [("tabS", i)], scale=col(C_SGN))
                else:
                    ACT(tabC[i], yf, AF.Sin, ["rt_yf"], [("tabC", i)], scale=2.0 * math.pi)

        def make_partner(dst, src, skey, dkey):
            s4 = src.rearrange("p k (g j) -> p k g j", j=64)
            d4 = dst.rearrange("p k (g j) -> p k g j", j=64)
            CP("act", d4[:, :, :, 16:64], s4[:, :, :, 16:64], [skey], [dkey])
            CP("act", d4[:, :, :, 0:8], s4[:, :, :, 8:16], [skey], [dkey])
            CP("act", d4[:, :, :, 8:16], s4[:, :, :, 0:8], [skey], [dkey])

        def rope_prep(col0, h):
            si = take_piece(col0 + h * 128)
            make_partner(wpart[h % 2], wsl[si], ("wsl", si), ("wpart", h % 2))
            return si

        def rope_proj(si, h, ntile, dst_fn):
            pi = h % 2
            for i in range(ntile):
                tsl = slice(i * 512, (i + 1) * 512)
                ba, bb = nbank(), nbank()
                for kc in range(KC):
                    MM(PS(ba), wsl[si][:, kc, :], hnT[:, kc, tsl], kc == 0, kc == KC - 1,
                       [("wsl", si), ("hnT", i, kc)], [pk(ba)])
                for kc in range(KC):
                    MM(PS(bb), wpart[pi][:, kc, :], hnT[:, kc, tsl], kc == 0, kc == KC - 1,
                       [("wpart", pi), ("hnT", i, kc)], [pk(bb)])
                TT("dve", rt1[i], PS(ba), tabC[i], ALU.mult, [pk(ba), ("tabC", i)], [("rt1", i)])
                TT("dve", rt2[i], PS(bb), tabS[i], ALU.mult, [pk(bb), ("tabS", i)], [("rt2", i)])
                dap, dkey = dst_fn(i)
                TT("dve", dap, rt1[i], rt2[i], ALU.add, [("rt1", i), ("rt2", i)], [dkey])

        def rope_heads(col0, dst_of):
            si_next = rope_prep(col0, 0)
            for h in range(4):
                si = si_next
                if h + 1 < 4:
                    si_next = rope_prep(col0, h + 1)
                rope_proj(si, h, 2, dst_of(h))

        def v_proj(kb0):
            for ti in range(8):
                b = nbank()
                for kc in range(KC):
                    MM(PS(b), hnT[:, kc, ti * 128:(ti + 1) * 128], Wv[:, kc, :], kc == 0, kc == KC - 1,
                       [("hnT", ti // 4, kc), "Wv"], [pk(b)])
                CP("act", Vp[:, kb0 + ti, :, 0:128], PS(b).rearrange("p (h d) -> p h d", h=4), [pk(b)], [("Vp", kb0 + ti)])

        def conv_w_piece(which, cc):
            c0 = 1536 + which * 512 + cc * 128
            return take_piece(c0)

        for ph in range(4):
            is_pre = ph < 2
            hh = ph % 2
            xsrc = dr["x_pre"] if is_pre else dr["x_own"]
            posv = dr["pos_pre"] if is_pre else dr["pos_own"]
            t0 = hh * 1024
            kv0 = (0 if is_pre else NT) + t0
            for i in range(2):
                rope_tables(posv, t0 + i * 512, i)
            tm_norm_half(xsrc, t0)
            rope_heads(512, lambda h: (lambda i, h=h: (k_rot[:, h, kv0 + i * 512:kv0 + (i + 1) * 512], ("k_rot", h, (kv0 // 512) + i))))
            v_proj(kv0 // 128)
            if is_pre and hh == 1:
                for cc in range(4):
                    sc = conv_w_piece(1, cc)
                    sh = conv_w_piece(2, cc)
                    bc_, bh_ = nbank(), nbank()
                    for kc in range(KC):
                        MM(PS(bc_)[:, 0:2], wsl[sc][:, kc, :], hnT[:, kc, 1022:1024], kc == 0, kc == KC - 1,
                           [("wsl", sc), ("hnT", 1, kc)], [pk(bc_)])
                    for kc in range(KC):
                        MM(PS(bh_)[:, 0:2], wsl[sh][:, kc, :], hnT[:, kc, 1022:1024], kc == 0, kc == KC - 1,
                           [("wsl", sh), ("hnT", 1, kc)], [pk(bh_)])
                    CP("act", csb[0][:, 0:2], PS(bc_)[:, 0:2], [pk(bc_)], [("csb", 0)])
                    TT("dve", ubuf[cc][:, 0:2], PS(bh_)[:, 0:2], csb[0][:, 0:2], ALU.mult, [pk(bh_), ("csb", 0)], [("u", cc)])
            if not is_pre:
                rope_heads(0, lambda h: (lambda i, h=h: (q_rot[:, h, t0 + i * 512:t0 + (i + 1) * 512], ("q_rot", h, hh * 2 + i))))
                for cc in range(4):
                    sb_ = conv_w_piece(0, cc)
                    sc = conv_w_piece(1, cc)
                    sh = conv_w_piece(2, cc)
                    for i in range(2):
                        tsl = slice(i * 512, (i + 1) * 512)
                        gt = hh * 2 + i
                        bb_, bc_, bh_ = nbank(), nbank(), nbank()
                        for (bk, sl_) in ((bc_, sc), (bh_, sh), (bb_, sb_)):
                            for kc in range(KC):
                                MM(PS(bk), wsl[sl_][:, kc, :], hnT[:, kc, tsl], kc == 0, kc == KC - 1,
                                   [("wsl", sl_), ("hnT", i, kc)], [pk(bk)])
                        j = i % 2
                        CP("act", csb[j], PS(bc_), [pk(bc_)], [("csb", j)])
                        u = ubuf[cc]
                        TT("dve", u[:, 2:514], PS(bh_), csb[j], ALU.mult, [pk(bh_), ("csb", j)], [("u", cc)])
                        y = ybuf[j]
                        cw = convw.rearrange("p (c k) -> p c k", k=3)
                        S.op("act", lambda e, y=y, u=u, cw=cw, cc=cc: e.mul(out=y, in_=u[:, 2:514], mul=cw[:, cc, 2:3]), [("u", cc), "convw"], [("y", j)])
                        STT("pool", y, u[:, 1:513], cw[:, cc, 1:2], y, ALU.mult, ALU.add, [("u", cc), "convw", ("y", j)], [("y", j)])
                        STT("pool", y, u[:, 0:512], cw[:, cc, 0:1], y, ALU.mult, ALU.add, [("u", cc), "convw", ("y", j)], [("y", j)])
                        TT("dve", gconvT[:, cc, gt * 512:(gt + 1) * 512], PS(bb_), y, ALU.mult, [pk(bb_), ("y", j)], [("gconvT", cc, gt)])
                        CP("pool", small[:, 16 + 2 * cc:18 + 2 * cc], u[:, 512:514], [("u", cc)], [("uh", cc)])
                        CP("pool", u[:, 0:2], small[:, 16 + 2 * cc:18 + 2 * cc], [("uh", cc)], [("u", cc)])

        acc = [None, None]
        for c in range(2):
            acc[c] = [PS(4 + 2 * c + j // 2)[:, (j % 2) * 256:(j % 2) * 256 + 129] for j in range(4)]
        S.barrier()
        accS = [arena[:, rt_base + c * 1024:rt_base + (c + 1) * 1024].rearrange("p (j d) -> p j d", j=4) for c in range(2)]
        steps = []
        for h in range(4):
            for qt in range(4):
                nown = 4 * qt + 4
                kbs = list(range(16)) + [16 + k for k in range(nown)]
                for ki, kb in enumerate(kbs):
                    steps.append(dict(h=h, qt=qt, kb=kb, first=(ki == 0), lastkb=(ki == len(kbs) - 1)))

        def st_geom(sp):
            is_own = sp["kb"] >= 16
            ko = sp["kb"] - 16
            j0 = max(0, ko - 4 * sp["qt"]) if is_own else 0
            return is_own, ko, j0, 512 - 128 * j0, sp["qt"] * 512 + 128 * j0

        def emit_st(k):
            sp = steps[k]
            is_own, ko, j0, ncol, q0 = st_geom(sp)
            h, kb = sp["h"], sp["kb"]
            pb = k % 2
            for c in range(2):
                psl = slice(64 * c, 64 * c + 64)
                MM(PS(2 * pb + c)[:, 0:ncol], k_rot[psl, h, kb * 128:(kb + 1) * 128], q_rot[psl, h, q0:q0 + ncol], True, True,
                   [("k_rot", h, kb // 4), ("q_rot", h, sp["qt"])], [pk(2 * pb + c)])

        def epilogue(h, qt):
            for c in range(2):
                for hb in range(2):
                    bz = 4 + 2 * c + hb
                    CP("dve", accS[c][:, 2 * hb:2 * hb + 2, :].rearrange("p j d -> p (j d)"), PS(bz), [pk(bz)], [("accS", c, hb)])
            ssq = small[:, 40:44]
            rsq = small[:, 44:48]
            for j in range(4):
                oi = j % 2
                ka = [("accS", 0, j // 2), ("accS", 1, j // 2)]
                sc_ = small[:, 32 + 4 * oi:36 + 4 * oi]
                sk = ("sc", oi)
                RECIP(sc_[:, 0:1], accS[0][:, j, 128:129], ka, [sk])
                RECIP(sc_[:, 1:2], accS[1][:, j, 128:129], ka, [sk])
                TT("dve", sc_[:, 1:2], sc_[:, 1:2], neglam, ALU.mult, [sk, "neglam"], [sk])
                TS("dve", obuf[oi], accS[0][:, j, 0:128], sc_[:, 0:1], None, ALU.mult, None, ka + [sk], [("ob", oi)])
                STT("dve", accS[0][:, j, 0:128], accS[1][:, j, 0:128], sc_[:, 1:2], obuf[oi], ALU.mult, ALU.add,
                    ka + [sk, ("ob", oi)], [("accS", 0, j // 2)])
                TT("dve", obuf[oi], accS[0][:, j, 0:128], accS[0][:, j, 0:128], ALU.mult, [("accS", 0, j // 2)], [("ob", oi)])
                S.op("dve", lambda e, oi=oi, j=j: e.reduce_sum(out=ssq[:, j:j + 1], in_=obuf[oi], axis=AX.X), [("ob", oi)], ["ssq"])
            ACT(rsq, ssq, AF.Ln, ["ssq", "cst"], ["rsq"], scale=1.0 / 128, bias=col(C_EPS))
            ACT(rsq, rsq, AF.Exp, ["rsq"], ["rsq"], scale=-0.5)
            for j in range(4):
                oi = j % 2
                STT("dve", onb[oi], accS[0][:, j, 0:128], rsq[:, j:j + 1], subg, ALU.mult, ALU.mult,
                    [("accS", 0, j // 2), "rsq", "subg"], [("onb", oi)])
                TR(PSB(3)[:, 0:128], onb[oi], identb, [("onb", oi), "identb"], [pk(3)])
                CP("dve", q_rot[:, h, qt * 512 + j * 128:qt * 512 + (j + 1) * 128], PSB(3)[:, 0:128], [pk(3)], [("q_rot", h, qt)])

        emit_st(0)
        emit_st(1)
        for k, sp in enumerate(steps):
            is_own, ko, j0, ncol, q0 = st_geom(sp)
            h, qt, kb = sp["h"], sp["qt"], sp["kb"]
            pb = k % 2
            pi_ = k % 3
            if sp["first"]:
                for bz in (4, 5, 6, 7):
                    MEMSET("dve", PS(bz), 0.0, [pk(bz)])
            stv = pspair[pb][:, :].rearrange("p (c n) -> p c n", c=2)
            ACT(PT[pi_][:, :, 0:ncol], stv[:, :, 0:ncol], AF.Exp, [pk(2 * pb), pk(2 * pb + 1), "cst"], [("PT", pi_)],
                scale=0.125, bias=(col(C_ZERO) if is_own else col(C_PMASK)))
            if is_own and ko >= 4 * qt:
                for c in range(2):
                    TT("dve", PT[pi_][:, c, 0:128], PT[pi_][:, c, 0:128], trib, ALU.mult, [("PT", pi_), "trib"], [("PT", pi_)])
            if k + 2 < len(steps):
                emit_st(k + 2)
            for c in range(2):
                for j in range(j0, 4):
                    last = (kb == 16 + 4 * qt + j)
                    bkey = pk(4 + 2 * c + j // 2)
                    MM(acc[c][j], PT[pi_][:, c, (j - j0) * 128:(j - j0 + 1) * 128], Vp[:, kb, h, 0:129], False, last,
                       [("PT", pi_), ("Vp", kb), "Vp_ones"], [bkey], skip=True)
            if sp["lastkb"]:
                epilogue(h, qt)

        S.barrier()
        attnT = q_rot
        top[0] = l0_wtop
        xrow = [carve(D) for _ in range(4)]
        for tt in range(4):
            for r in range(4):
                DMA("sp", xrow[r], dr["x_own"][tt * 512 + r * 128:tt * 512 + (r + 1) * 128, :], [], [("xrow", r)])
            for dm in range(KC):
                si = wslot()
                load_w_piece(wsl[si], dr["a_w_out"][:, dm * 128:(dm + 1) * 128], ("wsl", si))
                bx, bo = nbank(), nbank()
                for r in range(4):
                    TR(PS(bx)[:, r * 128:(r + 1) * 128], xrow[r][:, dm * 128:(dm + 1) * 128], identf, [("xrow", r), "cst"], [pk(bx)])
                CP("act", hT[:, dm, tt * 512:(tt + 1) * 512], PS(bx), [pk(bx)], [hk(dm, tt)])
                for kc in range(KC):
                    rhs = attnT[:, kc, tt * 512:(tt + 1) * 512] if kc < 4 else gconvT[:, kc - 4, tt * 512:(tt + 1) * 512]
                    rk = ("q_rot", kc, tt) if kc < 4 else ("gconvT", kc - 4, tt)
                    MM(PS(bo), wsl[si][:, kc, :], rhs, kc == 0, kc == KC - 1, [("wsl", si), rk], [pk(bo)])
                TT("dve", hT[:, dm, tt * 512:(tt + 1) * 512], PS(bo), hT[:, dm, tt * 512:(tt + 1) * 512], ALU.add,
                   [pk(bo), hk(dm, tt)], [hk(dm, tt)])
        S.barrier()
        top[0] = top_phase

        scr_sq = carve(KC * 512, BF16).rearrange("p (k t) -> p k t", k=KC)
        scr_r = carve(512)
        w13_base = top[0]
        w13 = [carve(KC * 128, BF16).rearrange("p (k n) -> p k n", k=KC) for _ in range(6)]
        w2s_base = top[0]
        w2s = [carve(NF * 128, BF16).rearrange("p (f n) -> p f n", f=NF) for _ in range(2)]
        sil = [carve(512) for _ in range(2)]
        gen_top = top[0]
        gT_region = carve(NF * 512)
        gT = gT_region.bitcast(BF16).rearrange("p (f t) -> p f t", f=NF)
        after_gT = top[0]
        top[0] = gen_top
        pT = carve(2 * 1024, BF16).rearrange("p (k t) -> p k t", k=2)
        prow = [carve(256) for _ in range(2)]
        prow_b = [carve(256, BF16) for _ in range(2)]
        sg = [carve(512) for _ in range(2)]
        w13rr = [0]
        w2rr = [0]

        def swiglu_group(w1, w3, w2, g0, post):
            def load_w2(dm):
                s2 = (w2rr[0] + dm) % 2
                DMA("pool", w2s[s2], w2[:, dm * 128:(dm + 1) * 128].rearrange("(f p) n -> p f n", p=128), [], [("w2s", s2)])

            for f in range(NF):
                s1 = w13rr[0] % 6
                s3 = (w13rr[0] + 1) % 6
                w13rr[0] += 2
                load_w_piece(w13[s1], w1[:, f * 128:(f + 1) * 128], ("w13", s1))
                load_w_piece(w13[s3], w3[:, f * 128:(f + 1) * 128], ("w13", s3))
                if f == NF - 4:
                    load_w2(0)
                if f == NF - 2:
                    load_w2(1)
                for i in range(2):
                    tsl = slice(i * 512, (i + 1) * 512)
                    ba, bb = nbank(0, 6), nbank(0, 6)
                    for kc in range(KC):
                        MM(PS(ba), w13[s1][:, kc, :], hnT[:, kc, tsl], kc == 0, kc == KC - 1, [("w13", s1), ("hnT", i, kc)], [pk(ba)])
                    for kc in range(KC):
                        MM(PS(bb), w13[s3][:, kc, :], hnT[:, kc, tsl], kc == 0, kc == KC - 1, [("w13", s3), ("hnT", i, kc)], [pk(bb)])
                    ACT(sil[i], PS(ba), AF.Silu, [pk(ba)], [("sil", i)])
                    TT("dve", gT[:, f, tsl], PS(bb), sil[i], ALU.mult, [pk(bb), ("sil", i)], [("gT", f, i)])
            for dm in range(KC):
                s2 = (w2rr[0] + dm) % 2
                for i in range(2):
                    tsl = slice(i * 512, (i + 1) * 512)
                    b = nbank(6, 8)
                    for f in range(NF):
                        MM(PS(b), w2s[s2][:, f, :], gT[:, f, tsl], f == 0, f == NF - 1, [("w2s", s2), ("gT", f, i)], [pk(b)])
                    post(b, dm, g0 * 2 + i)
                if dm + 2 < KC:
                    load_w2(dm + 2)

        def add_to_h(b, dm, tt):
            TT("dve", hT[:, dm, tt * 512:(tt + 1) * 512], PS(b), hT[:, dm, tt * 512:(tt + 1) * 512], ALU.add,
               [pk(b), hk(dm, tt)], [hk(dm, tt)])

        def ple(layer):
            gi = 4 + layer
            for g in range(2):
                for ti in range(8):
                    bi = ti % 2
                    DMA("sp", prow[bi], dr["p_own"][layer, g * 1024 + ti * 128:g * 1024 + (ti + 1) * 128, :], [], [("prow", bi)])
                    CP("dve", prow_b[bi], prow[bi], [("prow", bi)], [("prow_b", bi)])
                    b = nbank(0, 6)
                    for k2 in range(2):
                        TR(PSB(b)[:, k2 * 128:(k2 + 1) * 128], prow_b[bi][:, k2 * 128:(k2 + 1) * 128], identb, [("prow_b", bi), "identb"], [pk(b)])
                    CP("act", pT[:, :, ti * 128:(ti + 1) * 128], PSB(b)[:, 0:256].rearrange("p (k t) -> p k t", k=2), [pk(b)], [("pT", ti // 4)])
                fm_norm(gi, g * 2, 2, scr_sq, scr_r)
                for dm in range(KC):
                    sgw = w13rr[0] % 6
                    spw = (w13rr[0] + 1) % 6
                    w13rr[0] += 2
                    load_w_piece(w13[sgw], dr["ple_gate"][layer][:, dm * 128:(dm + 1) * 128], ("w13", sgw))
                    load_w_piece(w13[spw][:, 0:2, :], dr["ple_proj"][layer][:, dm * 128:(dm + 1) * 128], ("w13", spw))
                    for i in range(2):
                        tsl = slice(i * 512, (i + 1) * 512)
                        tt = g * 2 + i
                        bg, bp = nbank(0, 6), nbank(0, 6)
                        for kc in range(KC):
                            MM(PS(bg), w13[sgw][:, kc, :], hnT[:, kc, tsl], kc == 0, kc == KC - 1, [("w13", sgw), ("hnT", i, kc)], [pk(bg)])
                        for k2 in range(2):
                            MM(PS(bp), w13[spw][:, k2, :], pT[:, k2, tsl], k2 == 0, k2 == 1, [("w13", spw), ("pT", i)], [pk(bp)])
                        ACT(sg[i], PS(bg), AF.Sigmoid, [pk(bg)], [("sg", i)])
                        TT("dve", sg[i], PS(bp), sg[i], ALU.mult, [pk(bp), ("sg", i)], [("sg", i)])
                        TT("dve", hT[:, dm, tt * 512:(tt + 1) * 512], hT[:, dm, tt * 512:(tt + 1) * 512], sg[i], ALU.add,
                           [("sg", i), hk(dm, tt)], [hk(dm, tt)])

        def dump_h():
            pass

        if stop != "l0mix":
            for g in range(2):
                fm_norm(2, g * 2, 2, scr_sq, scr_r)
                swiglu_group(dr["ffn_w1"], dr["ffn_w3"], dr["ffn_w2"], g, add_to_h)
            S.barrier()
        if stop not in ("l0mix", "l0ffn"):
            ple(0)
            S.barrier()

        if stop not in ("l0mix", "l0ffn", "l0ple"):
            top[0] = gen_top
            uT = carve(KC * 1024, BF16).rearrange("p (k t) -> p k t", k=KC)
            suT = uT
            vvn = carve(8 * 1024, BF16).rearrange("p (i n) -> p i n", i=8)
            wsT = carve(8 * 128, BF16).rearrange("p (g t) -> p g t", g=8)
            wsf = carve(128)
            wsb = carve(128, BF16)
            bs_bc = carve(1024)
            lng_bc = carve(1024)
            lnb_bc = carve(1024)
            wcv = [carve(KC * 512, BF16).rearrange("p (k n) -> p k n", k=KC) for _ in range(2)]
            vraw = [carve(1024) for _ in range(2)]
            vtmp = [carve(1024) for _ in range(2)]
            vjunk = carve(1024, BF16)
            vst = [carve(8) for _ in range(2)]
            vsbig = vtmp
            DMA("sp", bs_bc, dr["c_b_s"].partition_broadcast(128), [], ["bs_bc"])
            DMA("sp", lng_bc, dr["c_ln_g"].partition_broadcast(128), [], ["lng_bc"])
            DMA("sp", lnb_bc, dr["c_ln_b"].partition_broadcast(128), [], ["lnb_bc"])
            for g8 in range(8):
                DMA("sp", wsf, dr["c_w_s"][g8], ["wsf"], ["wsf"])
                CP("dve", wsb, wsf, ["wsf"], ["wsb"])
                TR(PSB(0)[:, 0:128], wsb, identb, ["wsb", "identb"], [pk(0)])
                TT("dve", wsT[:, g8, :], PSB(0)[:, 0:128], trib, ALU.mult, [pk(0), "trib"], ["wsT"])
            for g in range(2):
                fm_norm(1, g * 2, 2, scr_sq, scr_r)
                for fc in range(KC):
                    s1 = w13rr[0] % 6
                    w13rr[0] += 1
                    load_w_piece(w13[s1], dr["c_w_in"][:, fc * 128:(fc + 1) * 128], ("w13", s1))
                    for i in range(2):
                        tsl = slice(i * 512, (i + 1) * 512)
                        b = nbank(0, 6)
                        for kc in range(KC):
                            MM(PS(b), w13[s1][:, kc, :], hnT[:, kc, tsl], kc == 0, kc == KC - 1, [("w13", s1), ("hnT", i, kc)], [pk(b)])
                        ACT(uT[:, fc, tsl], PS(b), AF.Gelu, [pk(b)], [("uT", fc, i)])
                for hv in range(2):
                    load_w_piece(wcv[hv], dr["c_w_in"][:, 1024 + hv * 512:1024 + (hv + 1) * 512], ("wcv", hv))
                for ti in range(8):
                    bi = ti % 2
                    for hv in range(2):
                        b = nbank(0, 6)
                        for kc in range(KC):
                            MM(PS(b), hnT[:, kc, ti * 128:(ti + 1) * 128], wcv[hv][:, kc, :], kc == 0, kc == KC - 1,
                               [("hnT", ti // 4, kc), ("wcv", hv)], [pk(b)])
                        ACT(vraw[bi][:, hv * 512:(hv + 1) * 512], PS(b), AF.Gelu, [pk(b)], [("vraw", bi)])
                    st = vst[bi]
                    sk = ("vst", bi)
                    S.op("dve", lambda e, bi=bi, st=st: e.reduce_sum(out=st[:, 0:1], in_=vraw[bi], axis=AX.X), [("vraw", bi)], [sk])
                    TS("dve", st[:, 1:2], st[:, 0:1], -1.0 / 1024, None, ALU.mult, None, [sk], [sk])
                    S.op("act", lambda e, bi=bi, st=st: e.add(out=vtmp[bi], in_=vraw[bi], add=st[:, 1:2]), [("vraw", bi), sk], [("vtmp", bi)])
                    ACT(vjunk, vtmp[bi], AF.Square, [("vtmp", bi)], ["vjunk", sk], accum_out=st[:, 2:3])
                    ACT(st[:, 3:4], st[:, 2:3], AF.Sqrt, [sk, "cst"], [sk], scale=1.0 / 1024, bias=col(C_LNEPS))
                    RECIP(st[:, 3:4], st[:, 3:4], [sk], [sk])
                    STT("dve", vtmp[bi], vtmp[bi], st[:, 3:4], lng_bc, ALU.mult, ALU.mult, [("vtmp", bi), sk, "lng_bc"], [("vtmp", bi)])
                    TT("dve", vvn[:, ti, :], vtmp[bi], lnb_bc, ALU.add, [("vtmp", bi), "lnb_bc"], [("vvn", ti)])
                for ti in range(8):
                    pp_ = ti % 2
                    i = ti // 4
                    for g8 in range(8):
                        bk = 2 * pp_ + g8 // 4
                        MM(PS(bk)[:, (g8 % 4) * 128:(g8 % 4 + 1) * 128], vvn[:, ti, g8 * 128:(g8 + 1) * 128], wsT[:, g8, :], True, True,
                           [("vvn", ti), "wsT"], [pk(bk)])
                    vb = vsbig[pp_]
                    TT("dve", vb, pspair[pp_][:, :], bs_bc, ALU.add, [pk(2 * pp_), pk(2 * pp_ + 1), "bs_bc"], [("vtmp", pp_)])
                    uv = uT[:, :, ti * 128:(ti + 1) * 128]
                    TT("dve", uv, vb.rearrange("p (g t) -> p g t", g=8), uv, ALU.mult,
                       [("vtmp", pp_)] + [("uT", g8, i) for g8 in range(8)], [("uT", g8, i) for g8 in range(8)])
                for dm in range(KC):
                    s1 = w13rr[0] % 6
                    w13rr[0] += 1
                    load_w_piece(w13[s1], dr["c_w_out"][:, dm * 128:(dm + 1) * 128], ("w13", s1))
                    for i in range(2):
                        tsl = slice(i * 512, (i + 1) * 512)
                        b = nbank(6, 8)
                        for kc in range(KC):
                            MM(PS(b), w13[s1][:, kc, :], suT[:, kc, tsl], kc == 0, kc == KC - 1, [("w13", s1), ("uT", kc, i)], [pk(b)])
                        add_to_h(b, dm, g * 2 + i)
            S.barrier()

        if stop not in ("l0mix", "l0ffn", "l0ple", "l1mix"):
            top[0] = gen_top
            NB = 15
            M1all = carve(128).rearrange("p (i e) -> p i e", e=8)
            M2all = carve(128).rearrange("p (i e) -> p i e", e=8)
            Call = carve(128).rearrange("p (i e) -> p i e", e=8)
            G1all = carve(16)
            G2all = carve(16)
            S1f = carve(16)
            S2f = carve(16)
            S1i = carve(16, I32)
            S2i = carve(16, I32)
            Msum = carve(8)
            Mi = [carve(8) for _ in range(2)]
            cntf = carve(8)
            cnti = carve(8, I32)
            padf = carve(8)
            psf = carve(8)
            pef = carve(8)
            ebf = carve(16)
            basef = carve(16)
            tq = [carve(8) for _ in range(2)]
            onesf = vrows
            rw = carve(KC * 8).rearrange("p (k e) -> p k e", k=KC)
            idxf = carve(33)
            idxi = [carve(33, I32) for _ in range(2)]
            base2f = carve(16)
            moe_base = top[0]
            hn_d = None
            xs_d = nc.dram_tensor("xs_scr", [NB * 512, D], BF16, kind="Internal").ap()
            ys_d = nc.dram_tensor("ys_scr", [NB * 512, D], F32, kind="Internal").ap()
            hnF = carve(KC * 1024).rearrange("p (k t) -> p k t", k=KC)
            hn_all = carve(16 * 1024, BF16).rearrange("p (i n) -> p i n", i=16)
            lg = [carve(8) for _ in range(2)]
            l2 = [carve(8) for _ in range(2)]
            rsm = [carve(8) for _ in range(2)]
            MEMSET("dve", Msum, 0.0, ["Msum"])
            MEMSET("dve", onesf, 1.0, ["onesf"])
            zt = carve(1024, BF16)
            MEMSET("dve", zt, 0.0, ["zt"])
            for zi_ in range(NB * 4):
                DMA("sp", xs_d[zi_ * 128:(zi_ + 1) * 128, :], zt, ["zt"], [("xsz", zi_)])
            xsz_keys = [("xsz", zi_) for zi_ in range(NB * 4)]
            DMA("sp", rw, dr["router_w"].rearrange("(k p) e -> p k e", p=128), [], ["rw"])
            LS = cst[:, C_LS:C_LS + 128]
            for g in range(2):
                fm_norm(3, g * 2, 2, scr_sq, scr_r, want_f32=hnF)
                for ti in range(8):
                    i = g * 8 + ti
                    bi = ti % 2
                    b = nbank(0, 6)
                    for kc in range(KC):
                        MM(PS(b)[:, 0:8], hnF[:, kc, ti * 128:(ti + 1) * 128], rw[:, kc, :], kc == 0, kc == KC - 1,
                           [("hnF", ti // 4, kc), "rw"], [pk(b)])
                    CP("dve", lg[bi], PS(b)[:, 0:8], [pk(b)], [("lg", bi)])
                    r = rsm[bi]
                    rk = ("rsm", bi)
                    mk1 = M1all[:, i, :]
                    mk2 = M2all[:, i, :]
                    S.op("dve", lambda e, r=r, bi=bi: e.reduce_max(out=r[:, 0:1], in_=lg[bi], axis=AX.X), [("lg", bi)], [rk])
                    TS("dve", mk1, lg[bi], r[:, 0:1], None, ALU.is_equal, None, [("lg", bi), rk], [("M1", i)])
                    STT("dve", l2[bi], mk1, -1e30, lg[bi], ALU.mult, ALU.add, [("M1", i), ("lg", bi)], [("l2", bi)])
                    S.op("dve", lambda e, r=r, bi=bi: e.reduce_max(out=r[:, 1:2], in_=l2[bi], axis=AX.X), [("l2", bi)], [rk])
                    TS("dve", mk2, l2[bi], r[:, 1:2], None, ALU.is_equal, None, [("l2", bi), rk], [("M2", i)])
                    TT("dve", r[:, 2:3], r[:, 0:1], r[:, 1:2], ALU.subtract, [rk], [rk])
                    ACT(G1all[:, i:i + 1], r[:, 2:3], AF.Sigmoid, [rk], [("G1", i)])
                    TS("dve", G2all[:, i:i + 1], G1all[:, i:i + 1], -1.0, 1.0, ALU.mult, ALU.add, [("G1", i)], [("G2", i)])
                    TT("dve", Mi[bi], mk1, mk2, ALU.add, [("M1", i), ("M2", i)], [("Mi", bi)])
                    b2 = nbank(0, 6)
                    MM(PS(b2)[:, 0:8], LS, Mi[bi], True, False, ["cst", ("Mi", bi)], [pk(b2)])
                    MM(PS(b2)[:, 0:8], onesf, Msum, False, True, ["onesf", "Msum"], [pk(b2)])
                    CP("dve", Call[:, i, :], PS(b2)[:, 0:8], [pk(b2)], [("Call", i)])
                    TT("dve", Msum, Msum, Mi[bi], ALU.add, ["Msum", ("Mi", bi)], ["Msum"])
                    b3 = nbank(0, 6)
                    for kc in range(KC):
                        TR(PSB(b3)[:, kc * 128:(kc + 1) * 128], hnT[:, kc, ti * 128:(ti + 1) * 128], identb,
                           [("hnT", ti // 4, kc), "identb"], [pk(b3)])
                    CP("act", hn_all[:, i, :], PSB(b3), [pk(b3)], [("hn_all", i)])
            b = nbank(0, 6)
            MM(PS(b)[:, 0:8], onesf, Msum, True, True, ["onesf", "Msum"], [pk(b)])
            CP("dve", cntf, PS(b)[:, 0:8], [pk(b)], ["cntf"])
            MEMSET("dve", padf, 0.0, ["padf"])
            for m_ in range(4):
                STT("dve", padf, cntf, 512.0 * m_, padf, ALU.is_gt, ALU.add, ["cntf", "padf"], ["padf"])
            TS("dve", padf, padf, 512.0, None, ALU.mult, None, ["padf"], ["padf"])
            MEMSET("dve", psf, 0.0, ["psf"])
            for e8 in range(1, NE):
                TT("dve", psf[:, e8:e8 + 1], psf[:, e8 - 1:e8], padf[:, e8 - 1:e8], ALU.add, ["psf", "padf"], ["psf"])
            TT("dve", pef, psf, padf, ALU.add, ["psf", "padf"], ["pef"])
            for i in range(16):
                bi = i % 2
                TT("dve", tq[bi], Call[:, i, :], psf, ALU.add, [("Call", i), "psf"], [("tq", bi)])
                TT("dve", lg[bi], tq[bi], M1all[:, i, :], ALU.mult, [("tq", bi), ("M1", i)], [("lg", bi)])
                S.op("dve", lambda e, i=i, bi=bi: e.reduce_sum(out=S1f[:, i:i + 1], in_=lg[bi], axis=AX.X), [("lg", bi)], ["S1f"])
                TT("dve", l2[bi], tq[bi], M2all[:, i, :], ALU.mult, [("tq", bi), ("M2", i)], [("l2", bi)])
                S.op("dve", lambda e, i=i, bi=bi: e.reduce_sum(out=S2f[:, i:i + 1], in_=l2[bi], axis=AX.X), [("l2", bi)], ["S2f"])
            CP("dve", S1i, S1f, ["S1f"], ["S1i"])
            CP("dve", S2i, S2f, ["S2f"], ["S2i"])
            MEMSET("dve", ebf, 0.0, ["ebf"])
            for bb in range(NB):
                bi = bb % 2
                TS("dve", tq[bi], pef, 512.0 * bb, None, ALU.is_le, None, ["pef"], [("tq", bi)])
                S.op("dve", lambda e, bb=bb, bi=bi: e.reduce_sum(out=ebf[:, bb:bb + 1], in_=tq[bi], axis=AX.X), [("tq", bi)], ["ebf"])
            TS("dve", ebf, ebf, 7.0, 2816.0, ALU.min, ALU.mult, ["ebf"], ["ebf"])
            TS("dve", basef, ebf, col(C_PIDX), None, ALU.add, None, ["ebf", "cst"], ["basef"])
            TS("dve", base2f, ebf, 0.5, None, ALU.mult, None, ["ebf"], ["base2f"])
            TS("dve", base2f, base2f, col(C_PIDX), None, ALU.add, None, ["base2f", "cst"], ["base2f"])
            for i in range(16):
                S.op("pool", lambda e, i=i: e.indirect_dma_start(out=xs_d, out_offset=bass.IndirectOffsetOnAxis(ap=S1i[:, i:i + 1], axis=0),
                                                                 in_=hn_all[:, i, :], in_offset=None),
                     [("hn_all", i), "S1i"] + xsz_keys, [("xs", i, 0)], dma=True)
                S.op("pool", lambda e, i=i: e.indirect_dma_start(out=xs_d, out_offset=bass.IndirectOffsetOnAxis(ap=S2i[:, i:i + 1], axis=0),
                                                                 in_=hn_all[:, i, :], in_offset=None),
                     [("hn_all", i), "S2i"] + xsz_keys, [("xs", i, 1)], dma=True)
            S.barrier()
            xs_keys = [("xs", i, w_) for i in range(16) for w_ in range(2)]
            top[0] = moe_base
            XsT = carve(KC * 512, BF16).rearrange("p (k t) -> p k t", k=KC)
            xs_tm = [carve(1024, BF16) for _ in range(4)]
            gTb = carve(NF * 512, BF16).rearrange("p (f t) -> p f t", f=NF)
            w13p = [arena[:, w13_base + k * 1024:w13_base + (k + 1) * 1024].bitcast(BF16).rearrange("p (w k n) -> p w k n", w=2, k=KC) for k in range(3)]
            w2p = [arena[:, w2s_base + k * 1024:w2s_base + (k + 1) * 1024].bitcast(BF16).rearrange("p (w d) -> p w d", w=2) for k in range(2)]
            w2p.append(carve(2048, BF16).rearrange("p (w d) -> p w d", w=2))
            stage = [carve(2048) for _ in range(3)]
            ystage = [carve(1024) for _ in range(2)]
            w13r = dr["moe_w13"]
            w2f = dr["moe_w2"]
            stg = [0]
            w13p_rr = [0]
            w2r_rr = [0]
            ys_rr = [0]

            def gather_piece(src, ii, colidx, dst_bf, dst_key, cast_eng):
                sl = stg[0] % 3
                stg[0] += 1
                S.op("pool", lambda e, sl=sl, ii=ii, colidx=colidx, src=src: e.indirect_dma_start(
                    out=stage[sl], out_offset=None, in_=src, in_offset=bass.IndirectOffsetOnAxis(ap=idxi[ii][:, colidx:colidx + 1], axis=0)),
                    [("idxi", ii)], [("stage", sl)], dma=True)
                CP(cast_eng, dst_bf, stage[sl], [("stage", sl)], [dst_key])

            XsT2 = [XsT, carve(KC * 512, BF16).rearrange("p (k t) -> p k t", k=KC)]

            def make_idx(bb):
                ii = bb % 2
                TS("dve", idxf[:, 0:22], cst[:, C_F128:C_F128 + 22], basef[:, bb:bb + 1], None, ALU.add, None, ["cst", "basef"], ["idxf"])
                TS("dve", idxf[:, 22:33], cst[:, C_F128:C_F128 + 11], base2f[:, bb:bb + 1], None, ALU.add, None, ["cst", "base2f"], ["idxf"])
                CP("dve", idxi[ii], idxf, ["idxf"], [("idxi", ii)])

            def load_xs(bb):
                for j in range(4):
                    DMA("sp", xs_tm[j], xs_d[bb * 512 + j * 128:bb * 512 + (j + 1) * 128, :], xs_keys, [("xs_tm", j)])

            def transpose_xs(bb):
                xt_ = XsT2[bb % 2]
                for j in range(4):
                    b3 = nbank(0, 6)
                    for kc in range(KC):
                        TR(PSB(b3)[:, kc * 128:(kc + 1) * 128], xs_tm[j][:, kc * 128:(kc + 1) * 128], identb, [("xs_tm", j), "identb"], [pk(b3)])
                    CP("dve", xt_[:, :, j * 128:(j + 1) * 128], PSB(b3).rearrange("p (k t) -> p k t", k=KC), [pk(b3)], [("XsT", bb % 2, j)])

            def fetch13(bb, f):
                sp_ = (bb * NF + f) % 3
                gather_piece(w13r, bb % 2, f, w13p[sp_].rearrange("p w k n -> p (w k n)"), ("w13p", sp_), "act")

            def fetch2(bb, fp):
                sr = (bb * 11 + fp) % 3
                gather_piece(w2f, bb % 2, 22 + fp, w2p[sr].rearrange("p w d -> p (w d)"), ("w2p", sr), "dve")

            make_idx(0)
            load_xs(0)
            transpose_xs(0)
            fetch13(0, 0)
            for bb in range(NB):
                xt_ = XsT2[bb % 2]
                xk = [("XsT", bb % 2, j) for j in range(4)]
                if bb + 1 < NB:
                    make_idx(bb + 1)
                bank_rr[0] = 0
                for f in range(NF):
                    sp_ = (bb * NF + f) % 3
                    if f + 1 < NF:
                        fetch13(bb, f + 1)
                    else:
                        fetch2(bb, 0)
                    if f == 8 and bb + 1 < NB:
                        load_xs(bb + 1)
                    ba, bb_ = nbank(0, 6), nbank(0, 6)
                    for kc in range(KC):
                        MM(PS(ba), w13p[sp_][:, 0, kc, :], xt_[:, kc, :], kc == 0, kc == KC - 1, [("w13p", sp_)] + xk, [pk(ba)])
                    for kc in range(KC):
                        MM(PS(bb_), w13p[sp_][:, 1, kc, :], xt_[:, kc, :], kc == 0, kc == KC - 1, [("w13p", sp_)] + xk, [pk(bb_)])
                    ACT(sil[f % 2], PS(ba), AF.Silu, [pk(ba)], [("sil", f % 2)])
                    TT("dve", gTb[:, f, :], PS(bb_), sil[f % 2], ALU.mult, [pk(bb_), ("sil", f % 2)], [("gTb", f)])
                if bb + 1 < NB:
                    transpose_xs(bb + 1)
                for fp in range(11):
                    sr = (bb * 11 + fp) % 3
                    if fp + 1 < 11:
                        fetch2(bb, fp + 1)
                    elif bb + 1 < NB:
                        fetch13(bb + 1, 0)
                    for two in range(2):
                        f = 2 * fp + two
                        for j in range(4):
                            for hf in range(2):
                                bk = j * 2 + hf
                                MM(PS(bk), gTb[:, f, j * 128:(j + 1) * 128], w2p[sr][:, two, hf * 512:(hf + 1) * 512], f == 0, f == NF - 1,
                                   [("gTb", f), ("w2p", sr)], [pk(bk)])
                for j in range(4):
                    yi = ys_rr[0] % 2
                    ys_rr[0] += 1
                    CP("act", ystage[yi][:, 0:512], PS(j * 2), [pk(j * 2)], [("ystage", yi)])
                    CP("dve", ystage[yi][:, 512:1024], PS(j * 2 + 1), [pk(j * 2 + 1)], [("ystage", yi)])
                    DMA("sp", ys_d[bb * 512 + j * 128:bb * 512 + (j + 1) * 128, :], ystage[yi], [("ystage", yi)], [("ys", bb, j)])
            S.barrier()
            ys_keys = [("ys", bb, j) for bb in range(NB) for j in range(4)]
            ypair = [(stage[0][:, 0:1024], stage[0][:, 1024:2048]), (stage[2][:, 0:1024], stage[2][:, 1024:2048])]
            zb = [stage[1][:, 0:1024], stage[1][:, 1024:2048]]
            for i in range(16):
                zi = i % 2
                y1, y2 = ypair[zi]
                S.op("pool", lambda e, i=i, y1=y1: e.indirect_dma_start(out=y1, out_offset=None, in_=ys_d, in_offset=bass.IndirectOffsetOnAxis(ap=S1i[:, i:i + 1], axis=0)),
                     ys_keys + ["S1i"], [("y1", zi)], dma=True)
                S.op("pool", lambda e, i=i, y2=y2: e.indirect_dma_start(out=y2, out_offset=None, in_=ys_d, in_offset=bass.IndirectOffsetOnAxis(ap=S2i[:, i:i + 1], axis=0)),
                     ys_keys + ["S2i"], [("y2", zi)], dma=True)
                TS("dve", zb[zi], y1, G1all[:, i:i + 1], None, ALU.mult, None, [("y1", zi), ("G1", i)], [("zb", zi)])
                STT("dve", zb[zi], y2, G2all[:, i:i + 1], zb[zi], ALU.mult, ALU.add, [("y2", zi), ("G2", i), ("zb", zi)], [("zb", zi)])
                for half in range(2):
                    b = nbank(0, 6)
                    for k4 in range(4):
                        kc = half * 4 + k4
                        TR(PS(b)[:, k4 * 128:(k4 + 1) * 128], zb[zi][:, kc * 128:(kc + 1) * 128], identf, [("zb", zi), "cst"], [pk(b)])
                    tt_ = i // 4
                    hv = hT[:, half * 4:(half + 1) * 4, i * 128:(i + 1) * 128]
                    TT("dve", hv, PS(b).rearrange("p (k t) -> p k t", k=4), hv, ALU.add,
                       [pk(b)] + [hk(half * 4 + k4, tt_) for k4 in range(4)], [hk(half * 4 + k4, tt_) for k4 in range(4)])
            S.barrier()

        if stop not in ("l0mix", "l0ffn", "l0ple", "l1mix", "l1moe"):
            ple(1)
            S.barrier()

        top[0] = after_gT
        fin = carve(KC * 512).rearrange("p (k t) -> p k t", k=KC)
        orow = [carve(D) for _ in range(2)]
        final = stop is None
        for tt in range(4):
            tsl = slice(tt * 512, (tt + 1) * 512)
            hkeys = [hk(dm, tt) for dm in range(KC)]
            if final:
                ACT(scr_sq, hT[:, :, tsl], AF.Square, hkeys, ["scr_sq"])
                b = nbank(0, 6)
                for kc in range(KC):
                    MM(PS(b), onesb, scr_sq[:, kc, :], kc == 0, kc == KC - 1, ["scr_sq", "onesb"], [pk(b)])
                ACT(scr_r, PS(b), AF.Sqrt, [pk(b)], ["scr_r"], scale=1.0 / D, bias=col(C_EPS))
                RECIP(scr_r, scr_r, ["scr_r"], ["scr_r"])
                for kc in range(KC):
                    eng = "dve" if kc % 2 == 0 else "pool"
                    STT(eng, fin[:, kc, :], hT[:, kc, tsl], gcols[:, 48 + kc:49 + kc], scr_r, ALU.mult, ALU.mult,
                        [hk(kc, tt), "gcols", "scr_r"], ["fin"])
                src = fin
                skeys = ["fin"]
            else:
                src = hT[:, :, tsl]
                skeys = hkeys
            for r in range(4):
                oi = (tt * 4 + r) % 2
                for half in range(2):
                    b = nbank(0, 6)
                    for k4 in range(4):
                        kc = half * 4 + k4
                        TR(PS(b)[:, k4 * 128:(k4 + 1) * 128], src[:, kc, r * 128:(r + 1) * 128], identf, skeys + ["cst"], [pk(b)])
                    CP("act" if half == 0 else "dve", orow[oi][:, half * 512:(half + 1) * 512], PS(b), [pk(b)], [("orow", oi)])
                DMA("sp", out_d[tt * 512 + r * 128:tt * 512 + (r + 1) * 128, :], orow[oi], [("orow", oi)], ["out"])

        block = es.enter_context(nc.Block())
        S.emit(nc, block, es)
        build.stats = S.stats
    return nc


def make_consts(half):
    c = np.zeros((128, NCONST), np.float32)
    c[:, C_ID:C_ID + 128] = np.eye(128, dtype=np.float32)
    p = np.arange(128)[:, None]
    f = np.arange(128)[None, :]
    c[:, C_TRI:C_TRI + 128] = (f >= p).astype(np.float32)
    rot_dim = 16
    inv_freq = (500000.0 ** (-np.arange(0, rot_dim, 2, dtype=np.float32) / rot_dim)).astype(np.float32)
    for pp in range(128):
        d = pp % 64
        if d < 16:
            c[pp, C_INVF] = inv_freq[d % 8] / (2.0 * math.pi)
            c[pp, C_SGN] = (-1.0 if d < 8 else 1.0) * 2.0 * math.pi
    c[:, C_PMASK] = 0.0 if half == 1 else -30000.0
    c[:, C_ZERO] = 0.0
    c[:, C_EPS] = 1e-6
    c[:, C_LNEPS] = 1e-5
    c[:, C_ONE] = 1.0
    c[:, C_QTR] = 0.25
    c[:, C_PIDX] = np.arange(128)
    c[:, C_F128:C_F128 + 22] = (np.arange(22) * 128)[None, :]
    c[:, C_LS:C_LS + 128] = (f > p).astype(np.float32)
    return c


def make_in_maps(inputs):
    x = np.asarray(inputs["x"], np.float32)
    p = np.asarray(inputs["p"], np.float32)
    pos = np.asarray(inputs["positions"], np.int32)
    shared = {}
    for name, shp in W_SPECS:
        if name == "moe_w13":
            w1t = np.asarray(inputs["moe_w1"], np.float32).reshape(8, 8, 128, 22, 128).transpose(0, 3, 2, 1, 4)
            w3t = np.asarray(inputs["moe_w3"], np.float32).reshape(8, 8, 128, 22, 128).transpose(0, 3, 2, 1, 4)
            a = np.stack([w1t, w3t], axis=3)
        elif name == "moe_w2":
            a = np.asarray(inputs[name], np.float32).reshape(8, 11, 2, 128, 1024).transpose(0, 1, 3, 2, 4)
        else:
            a = np.asarray(inputs[name], np.float32)
        shared[name] = np.ascontiguousarray(a.reshape(shp))
    in_maps = []
    for c in range(8):
        b, half = c // 2, c % 2
        sl = slice(half * NT, (half + 1) * NT)
        m = dict(shared)
        m["x_own"] = np.ascontiguousarray(x[b, sl])
        m["pos_own"] = np.ascontiguousarray(pos[b, sl])
        if half == 1:
            m["x_pre"] = np.ascontiguousarray(x[b, 0:NT])
            m["pos_pre"] = np.ascontiguousarray(pos[b, 0:NT])
        else:
            m["x_pre"] = np.zeros((NT, D), np.float32)
            m["pos_pre"] = np.zeros((NT,), np.int32)
        m["p_own"] = np.ascontiguousarray(p[:, b, sl])
        m["consts"] = make_consts(half)
        in_maps.append(m)
    return in_maps


def kernel(**inputs):
    nc = build()
    in_maps = make_in_maps(inputs)
    res = run_bass_kernel_spmd(nc, in_maps, core_ids=list(range(8)))
    out = np.zeros((4, 2 * NT, D), np.float32)
    for c in range(8):
        b, half = c // 2, c % 2
        out[b, half * NT:(half + 1) * NT] = res.results[c]["out"]
    return out
```
